# Optimizing a Trainium2 kernel written in Bass

```python
import jax, jax.numpy as jnp
from jax import lax
import numpy as np

D_MODEL = 1024
BATCH = 16
SEQ = 4096
DEPTH = 4

EPS = 1e-6
M_HEADS = 4
M_WIDTH = D_MODEL
M_V_DIM = M_WIDTH // M_HEADS
M_QK_DIM = M_V_DIM // 2
M_QK_WIDTH = M_HEADS * M_QK_DIM
M_CHUNK = 64
CONV_K = 4
A_HEAD_DIM = 64
A_WIDTH = D_MODEL // 2
A_HEADS = A_WIDTH // A_HEAD_DIM
DILATED_PATTERNS = ((128, 1), (512, 4), (2048, 16))
A_BLOCK = 128
D_MIX = M_WIDTH + A_WIDTH
IN_WIDTHS = (M_QK_WIDTH, M_QK_WIDTH, M_WIDTH, M_WIDTH, M_WIDTH, M_HEADS, M_HEADS,
             A_WIDTH, A_WIDTH, A_WIDTH, A_WIDTH)
N_IN = sum(IN_WIDTHS)

kernel_name = "hymba_mlstm_dilated_swa_trunk"


def rms_norm(x, g):
    xf = x.astype(jnp.float32)
    y = xf * lax.rsqrt(jnp.mean(xf * xf, axis=-1, keepdims=True) + EPS)
    return (y * g.astype(jnp.float32)).astype(x.dtype)


def causal_depthwise_conv(x, w, b):
    C = x.shape[-1]
    out = lax.conv_general_dilated(
        x, w[:, None, :], window_strides=(1,), padding=((CONV_K - 1, 0),),
        dimension_numbers=("NWC", "WIO", "NWC"), feature_group_count=C)
    return out + b


def mlstm_chunkwise(q, k, v, i_pre, f_pre):
    B, S, H, dk = q.shape
    dv = v.shape[-1]
    L = M_CHUNK
    nc = S // L
    f32 = jnp.float32

    def to_chunks(t):
        t = t.reshape((B, nc, L, H) + t.shape[3:])
        return jnp.moveaxis(t, (1, 3), (0, 2))

    qc = to_chunks(q.astype(f32) * (dk ** -0.5))
    kc = to_chunks(k.astype(f32))
    vc = to_chunks(v.astype(f32))
    ic = to_chunks(i_pre.astype(f32))
    lfc = to_chunks(jax.nn.log_sigmoid(f_pre.astype(f32)))
    causal = jnp.tril(jnp.ones((L, L), dtype=bool))

    def step(carry, xs):
        C, n, m = carry
        qb, kb, vb, ib, lfb = xs
        b = jnp.cumsum(lfb, axis=-1)
        log_intra = jnp.where(causal, b[..., :, None] - b[..., None, :] + ib[..., None, :], -jnp.inf)
        log_inter = b + m[..., None]
        m_t = jnp.maximum(log_inter, jnp.max(log_intra, axis=-1))
        w_intra = jnp.exp(log_intra - m_t[..., None])
        w_inter = jnp.exp(log_inter - m_t)
        s = jnp.einsum('bhtd,bhsd->bhts', qb, kb) * w_intra
        num = jnp.einsum('bhts,bhsv->bhtv', s, vb) + w_inter[..., None] * jnp.einsum('bhtd,bhdv->bhtv', qb, C)
        den = jnp.sum(s, axis=-1) + w_inter * jnp.einsum('bhtd,bhd->bht', qb, n)
        h = num / jnp.maximum(jnp.abs(den), jnp.exp(-m_t))[..., None]
        b_last = b[..., -1]
        log_state = b_last[..., None] - b + ib
        m_new = jnp.maximum(b_last + m, jnp.max(log_state, axis=-1))
        w_s = jnp.exp(log_state - m_new[..., None])
        decay = jnp.exp(b_last + m - m_new)
        C_new = decay[..., None, None] * C + jnp.einsum('bhs,bhsd,bhsv->bhdv', w_s, kb, vb)
        n_new = decay[..., None] * n + jnp.einsum('bhs,bhsd->bhd', w_s, kb)
        return (C_new, n_new, m_new), h

    init = (jnp.zeros((B, H, dk, dv), f32), jnp.zeros((B, H, dk), f32), jnp.zeros((B, H), f32))
    _, h = lax.scan(step, init, (qc, kc, vc, ic, lfc))
    return jnp.moveaxis(h, (0, 2), (1, 3)).reshape(B, S, H, dv)


def dilated_window_attention(q, k, v, window, dilation):
    B, H, S, hd = q.shape
    L = S // dilation
    J = window // dilation
    nb = -(-L // A_BLOCK)
    Lp = nb * A_BLOCK

    def to_sub(t):
        return jnp.swapaxes(t.reshape(B, H, L, dilation, hd), 2, 3)

    qs = jnp.pad(to_sub(q), ((0, 0), (0, 0), (0, 0), (0, Lp - L), (0, 0)))
    pad_kv = ((0, 0), (0, 0), (0, 0), (A_BLOCK, Lp - L), (0, 0))
    ks = jnp.pad(to_sub(k), pad_kv)
    vs = jnp.pad(to_sub(v), pad_kv)
    qb = qs.reshape(B, H, dilation, nb, A_BLOCK, hd)

    def kv_blocks(t):
        prev = t[..., :Lp, :].reshape(B, H, dilation, nb, A_BLOCK, hd)
        cur = t[..., A_BLOCK:, :].reshape(B, H, dilation, nb, A_BLOCK, hd)
        return jnp.concatenate([prev, cur], axis=-2)

    kb = kv_blocks(ks)
    vb = kv_blocks(vs)
    blk = jnp.arange(nb)[:, None, None]
    qi = jnp.arange(A_BLOCK)[None, :, None]
    kc = jnp.arange(2 * A_BLOCK)[None, None, :]
    delta = qi - kc + A_BLOCK
    k_pos = blk * A_BLOCK - A_BLOCK + kc
    mask = (delta >= 0) & (delta <= J) & (k_pos >= 0)

    s = jnp.einsum('bhrnqd,bhrnkd->bhrnqk', qb, kb, preferred_element_type=jnp.float32)
    s = jnp.where(mask, s, -jnp.inf)
    mx = jnp.max(s, axis=-1, keepdims=True)
    p = jnp.exp(s - mx)
    den = jnp.sum(p, axis=-1, keepdims=True)
    o = jnp.einsum('bhrnqk,bhrnkd->bhrnqd', p, vb.astype(jnp.float32)) / den
    lse = (mx + jnp.log(den))[..., 0]
    o = o.reshape(B, H, dilation, Lp, hd)[..., :L, :]
    o = jnp.swapaxes(o, 2, 3).reshape(B, H, S, hd)
    lse = lse.reshape(B, H, dilation, Lp)[..., :L]
    lse = jnp.swapaxes(lse, 2, 3).reshape(B, H, S)
    return o, lse


def hybrid_layer(x, norm_g, w_in, gate_b, conv_w, conv_b, m_norm_g, q_norm_g, k_norm_g, w_out):
    B, S, _ = x.shape
    h = rms_norm(x, norm_g)
    proj = jnp.einsum('bsd,dn->bsn', h, w_in)
    split_points = np.cumsum(IN_WIDTHS)[:-1]
    mq, mk, mv, mo, mz, mi, mf, aq, ak, av, az = jnp.split(proj, split_points, axis=-1)

    qk = jax.nn.silu(causal_depthwise_conv(jnp.concatenate([mq, mk], axis=-1), conv_w, conv_b))
    mq, mk = jnp.split(qk, 2, axis=-1)
    gates = jnp.concatenate([mi, mf], axis=-1) + gate_b
    gi, gf = jnp.split(gates, 2, axis=-1)
    hm = mlstm_chunkwise(mq.reshape(B, S, M_HEADS, M_QK_DIM), mk.reshape(B, S, M_HEADS, M_QK_DIM),
                         mv.reshape(B, S, M_HEADS, M_V_DIM), gi, gf)
    hm = jax.nn.sigmoid(mo.astype(jnp.float32)).reshape(B, S, M_HEADS, M_V_DIM) * hm
    hm = rms_norm(hm, m_norm_g.reshape(M_HEADS, M_V_DIM)).reshape(B, S, M_WIDTH)
    hm = hm.astype(x.dtype) * jax.nn.silu(mz)

    def heads(t, g):
        t = t.reshape(B, S, A_HEADS, A_HEAD_DIM)
        if g is not None:
            t = rms_norm(t, g)
        return jnp.transpose(t, (0, 2, 1, 3))
    qa = heads(aq, q_norm_g) * (A_HEAD_DIM ** -0.5)
    ka = heads(ak, k_norm_g)
    va = heads(av, None)
    outs, lses = [], []
    for window, dilation in DILATED_PATTERNS:
        o, lse = dilated_window_attention(qa, ka, va, window, dilation)
        outs.append(o)
        lses.append(lse)
    wts = jax.nn.softmax(jnp.stack(lses, axis=0), axis=0)
    ha = jnp.sum(wts[..., None] * jnp.stack(outs, axis=0), axis=0)
    ha = jnp.transpose(ha, (0, 2, 1, 3)).reshape(B, S, A_WIDTH).astype(x.dtype) * jax.nn.silu(az)

    y = jnp.einsum('bsm,md->bsd', jnp.concatenate([hm, ha], axis=-1), w_out)
    return x + y.astype(x.dtype)


def setup_inputs(seed: int = 0) -> dict:
    key = jax.random.key(seed)
    ks = jax.random.split(key, 12)
    f32 = jnp.float32
    x = jax.random.normal(ks[0], (BATCH, SEQ, D_MODEL), f32)
    norm_g = 1.0 + 0.02 * jax.random.normal(ks[1], (DEPTH, D_MODEL), f32)
    w_in = jax.random.normal(ks[2], (DEPTH, D_MODEL, N_IN), f32) * (D_MODEL ** -0.5)
    i_bias = 0.1 * jax.random.normal(ks[3], (DEPTH, M_HEADS), f32)
    f_bias = jnp.linspace(3.0, 6.0, M_HEADS, dtype=f32)[None, :] + 0.01 * jax.random.normal(ks[4], (DEPTH, M_HEADS), f32)
    gate_b = jnp.concatenate([i_bias, f_bias], axis=-1)
    conv_w = jax.random.normal(ks[5], (DEPTH, CONV_K, 2 * M_QK_WIDTH), f32) * (CONV_K ** -0.5)
    conv_b = 0.01 * jax.random.normal(ks[6], (DEPTH, 2 * M_QK_WIDTH), f32)
    m_norm_g = 1.0 + 0.02 * jax.random.normal(ks[7], (DEPTH, M_WIDTH), f32)
    q_norm_g = 1.0 + 0.02 * jax.random.normal(ks[8], (DEPTH, A_HEAD_DIM), f32)
    k_norm_g = 1.0 + 0.02 * jax.random.normal(ks[9], (DEPTH, A_HEAD_DIM), f32)
    w_out = jax.random.normal(ks[10], (DEPTH, D_MIX, D_MODEL), f32) * (D_MIX ** -0.5)
    return {"x": x, "norm_g": norm_g, "w_in": w_in, "gate_b": gate_b, "conv_w": conv_w,
            "conv_b": conv_b, "m_norm_g": m_norm_g, "q_norm_g": q_norm_g, "k_norm_g": k_norm_g,
            "w_out": w_out}


def reference(x, norm_g, w_in, gate_b, conv_w, conv_b, m_norm_g, q_norm_g, k_norm_g, w_out):
    for layer in range(DEPTH):
        x = hybrid_layer(x, norm_g[layer], w_in[layer], gate_b[layer], conv_w[layer], conv_b[layer],
                         m_norm_g[layer], q_norm_g[layer], k_norm_g[layer], w_out[layer])
    return x
```

```python
import numpy as np
import ml_dtypes
from contextlib import ExitStack
import concourse.bass as bass
import concourse.mybir as mybir
from concourse.bass_utils import run_bass_kernel_spmd

F32 = mybir.dt.float32
BF16 = mybir.dt.bfloat16
AF = mybir.ActivationFunctionType
ALU = mybir.AluOpType
AX = mybir.AxisListType
NPBF = ml_dtypes.bfloat16

D = 1024
NIN = 6152
DMIX = 1536
EPS = 1e-6
PATTERNS = (1, 4, 16)


class Sched:
    NDMA = 48

    def __init__(self, nc, es):
        self.nc = nc
        self.engs = {'pe': nc.tensor, 'act': nc.scalar, 'dve': nc.vector, 'pool': nc.gpsimd, 'sp': nc.sync}
        self.sems = {e: es.enter_context(nc.semaphore("sem_" + e)) for e in self.engs}
        self.cnt = {e: 0 for e in self.engs}
        self.known = {e: {} for e in self.engs}
        self.dma_sems = [es.enter_context(nc.semaphore("dsem%d" % i)) for i in range(self.NDMA)]
        self.dma_cnt = [0] * self.NDMA
        self.dma_rr = 0
        self.state = {}
        self.semobj = {}
        for e, s in self.sems.items():
            self.semobj["sem_" + e] = s
        for i, s in enumerate(self.dma_sems):
            self.semobj["dsem%d" % i] = s
        self.n_wait = 0
        self.n_ins = 0

    def _need(self, eng, tickets):
        kn = self.known[eng]
        own = "sem_" + eng
        best = {}
        for (sn, v) in tickets:
            if sn == own and (eng == 'pe' or v > self.cnt[eng]):
                continue
            if kn.get(sn, 0) >= v:
                continue
            if best.get(sn, 0) < v:
                best[sn] = v
        for sn, v in best.items():
            self.engs[eng].wait_ge(self.semobj[sn], v)
            kn[sn] = v
            self.n_wait += 1

    def _collect(self, eng, reads, writes):
        own = "sem_" + eng
        t = []
        for k in reads:
            st = self.state.get(k)
            if st:
                t.extend(st[0])
        for k in writes:
            st = self.state.get(k)
            if st:
                t.extend(st[0])
                t.extend(x for x in st[1] if x[0] != own)
        return t

    def _update(self, ticket, reads, writes):
        for k in reads:
            st = self.state.setdefault(k, ([], []))
            st[1].append(ticket)
            if len(st[1]) > 4096:
                best = {}
                for (sn, v) in st[1]:
                    if best.get(sn, 0) < v:
                        best[sn] = v
                st[1][:] = list(best.items())
        for k in writes:
            self.state[k] = ([ticket], [])

    def op(self, eng, fn, reads=(), writes=(), signal=True):
        self._need(eng, self._collect(eng, reads, writes))
        ins = fn(self.engs[eng])
        self.n_ins += 1
        sn = "sem_" + eng
        if signal:
            self.cnt[eng] += 1
            ins.then_inc(self.sems[eng], 1)
            ticket = (sn, self.cnt[eng])
        else:
            ticket = (sn, self.cnt[eng] + 1)
        self._update(ticket, reads, writes)
        return ticket

    def dma(self, eng, out, in_, reads=(), writes=(), **kw):
        i = self.dma_rr
        self.dma_rr = (self.dma_rr + 1) % self.NDMA
        sn = "dsem%d" % i
        tickets = self._collect(eng, reads, writes)
        if self.dma_cnt[i] > 0:
            tickets.append((sn, self.dma_cnt[i]))
        self._need(eng, tickets)
        ins = self.engs[eng].dma_start(out=out, in_=in_, **kw)
        self.n_ins += 1
        self.dma_cnt[i] += 16
        ins.then_inc(self.dma_sems[i], 16)
        ticket = (sn, self.dma_cnt[i])
        self._update(ticket, reads, writes)
        return ticket

    def barrier(self):
        allt = [("sem_" + e, self.cnt[e]) for e in self.engs if self.cnt[e] > 0]
        allt += [("dsem%d" % i, c) for i, c in enumerate(self.dma_cnt) if c > 0]
        for e in self.engs:
            self._need(e, allt)
        self.state = {}


class Rot:
    def __init__(self, items):
        self.items = items
        self.i = 0

    def next(self):
        it = self.items[self.i]
        self.i = (self.i + 1) % len(self.items)
        return it


def build_program(S_TOK, NSEQ, DEPTH, debug=False):
    assert S_TOK % 2048 == 0
    NTOK = S_TOK * NSEQ
    NT = S_TOK // 512
    NCH = S_TOK // 128
    nc = bass.Bass("TRN2", target_bir_lowering=False)

    def din(name, shape, dt):
        return nc.dram_tensor(name, shape, dt, kind="ExternalInput").ap()

    def dscr(name, shape, dt):
        return nc.dram_tensor(name, shape, dt, kind="Internal").ap()

    x_in = din("x", [NTOK, D], F32)
    w_in = din("w_in", [DEPTH, D, NIN], F32)
    w_g = din("w_g", [DEPTH, D, 256], F32)
    w_out = din("w_out", [DEPTH, DMIX, D], F32)
    normg = din("normg", [DEPTH, 128, 8], F32)
    convw = din("convw", [DEPTH, 128, 8, 4], F32)
    convb = din("convb", [DEPTH, 128, 8], F32)
    gateb = din("gateb", [DEPTH, 128, 2], F32)
    qkg = din("qkg", [DEPTH, 128, 2], F32)
    mng = din("mng", [DEPTH, 128, 1024], F32)
    c_identb = din("c_identb", [128, 128], BF16)
    c_identf = din("c_identf", [128, 128], F32)
    c_mask = din("c_mask", [128, 768], BF16)
    c_blk = din("c_blk", [128, 128], BF16)
    c_coef = din("c_coef", [128, 4], F32)
    y_out = nc.dram_tensor("y", [NTOK, D], F32, kind="ExternalOutput").ap()

    xs = [dscr("xs0", [NTOK, D], F32), dscr("xs1", [NTOK, D], F32)]
    ws_in = dscr("ws_in", [DEPTH, 128, 8 * 6144], BF16)
    ws_g = dscr("ws_g", [DEPTH, 128, 8 * 256], BF16)
    ws_out = dscr("ws_out", [DEPTH, 128, 12 * 1024], BF16)
    s_qT = dscr("s_qT", [NSEQ, 512, S_TOK], BF16)
    s_kT = dscr("s_kT", [NSEQ, 512, S_TOK], BF16)
    s_v = dscr("s_v", [NTOK, 1024], BF16)
    s_o = dscr("s_o", [NTOK, 1024], BF16)
    s_z = dscr("s_z", [NTOK, 1024], BF16)
    s_gI = dscr("s_gI", [NSEQ, 128, S_TOK], F32)
    s_gF = dscr("s_gF", [NSEQ, 128, S_TOK], F32)
    s_aqT = dscr("s_aqT", [NSEQ, 512, S_TOK], BF16)
    s_akT = dscr("s_akT", [NSEQ, 512, S_TOK], BF16)
    s_av = dscr("s_av", [NTOK, 512], BF16)
    s_az = dscr("s_az", [NTOK, 512], BF16)
    s_hm = dscr("s_hm", [NTOK, 1024], BF16)
    s_od = dscr("s_od", [3, NTOK, 520], F32)
    s_dec = dscr("s_dec", [128, NCH], F32)
    dbg = {}
    if debug:
        dbg["colz"] = nc.dram_tensor("dbg_colz", [128, NCH * 8], F32, kind="ExternalOutput").ap()
        dbg["decb"] = nc.dram_tensor("dbg_decb", [128, 4 * NCH], F32, kind="ExternalOutput").ap()

    with ExitStack() as top:
        S = Sched(nc, top)

        uniq = [0]

        def sbt(es, name, shape, dt):
            uniq[0] += 1
            return es.enter_context(nc.sbuf_tensor("%s_u%d" % (name, uniq[0]), shape, dt))

        def pst(es, name, shape, dt):
            uniq[0] += 1
            return es.enter_context(nc.psum_tensor("%s_u%d" % (name, uniq[0]), shape, dt))

        identb = sbt(top, "identb", [128, 128], BF16)
        identf = sbt(top, "identf", [128, 128], F32)
        maskt = sbt(top, "maskt", [128, 768], BF16)
        blk = sbt(top, "blk", [128, 128], BF16)
        coef = sbt(top, "coef", [128, 4], F32)
        colz = sbt(top, "colz", [128, NCH, 4, 2], F32)
        decb = sbt(top, "decb", [128, 4, NCH], F32)
        S.dma('sp', identb[:], c_identb[:, :], writes=["identb"])
        S.dma('sp', identf[:], c_identf[:, :], writes=["identf"])
        S.dma('sp', maskt[:], c_mask[:, :], writes=["maskt"])
        S.dma('sp', blk[:], c_blk[:, :], writes=["blk"])
        S.dma('sp', coef[:], c_coef[:, :], writes=["coef"])
        e0c, e1c, cstc, epsc = coef[:, 0:1], coef[:, 1:2], coef[:, 2:3], coef[:, 3:4]

        def weight_pieces(l):
            out = []
            for kc in range(8):
                rows = slice(kc * 128, (kc + 1) * 128)
                out.append((w_g[l, rows, :], ws_g[l, :, kc * 256:(kc + 1) * 256], 256, kc))
                out.append((w_in[l, rows, 4104:6152], ws_in[l, :, kc * 6144 + 4096:kc * 6144 + 6144], 2048, kc))
                out.append((w_in[l, rows, 0:2048], ws_in[l, :, kc * 6144:kc * 6144 + 2048], 2048, kc))
                out.append((w_in[l, rows, 2048:4096], ws_in[l, :, kc * 6144 + 2048:kc * 6144 + 4096], 2048, kc))
            for mc in range(12):
                out.append((w_out[l, mc * 128:(mc + 1) * 128, :], ws_out[l, :, mc * 1024:(mc + 1) * 1024], 1024, None))
            return out

        class Caster:
            def __init__(self, es, l, tag):
                self.l = l
                self.ng = sbt(es, tag + "_ng", [128, 8], F32)
                self.ngk = tag + "_ng"
                S.dma('sp', self.ng[:], normg[l], writes=[self.ngk])
                self.stg = Rot([(sbt(es, "%s_stg%d" % (tag, i), [128, 2048], F32), "%s_stg%d" % (tag, i)) for i in range(2)])
                self.obf = Rot([(sbt(es, "%s_obf%d" % (tag, i), [128, 2048], BF16), "%s_obf%d" % (tag, i)) for i in range(2)])
                self.k = 0
                self.pstore = None

            def flush(self):
                if self.pstore is not None:
                    (dst, ob, obk, n) = self.pstore
                    S.dma('sp', dst, ob[:, 0:n], reads=[obk])
                    self.pstore = None

            def run(self, piece):
                (src, dst, n, kc) = piece
                st, stk = self.stg.next()
                ob, obk = self.obf.next()
                S.dma('sp', st[:, 0:n], src, writes=[stk])
                self.flush()
                eng = 'dve' if self.k % 2 == 0 else 'act'
                self.k += 1
                if kc is None:
                    if eng == 'act':
                        S.op('act', lambda e: e.copy(out=ob[:, 0:n], in_=st[:, 0:n]), reads=[stk], writes=[obk])
                    else:
                        S.op('dve', lambda e: e.tensor_copy(out=ob[:, 0:n], in_=st[:, 0:n]), reads=[stk], writes=[obk])
                else:
                    gcol = self.ng[:, kc:kc + 1]
                    if eng == 'act':
                        S.op('act', lambda e: e.activation(out=ob[:, 0:n], in_=st[:, 0:n], func=AF.Copy, scale=gcol), reads=[stk, self.ngk], writes=[obk])
                    else:
                        S.op('dve', lambda e: e.tensor_scalar(out=ob[:, 0:n], in0=st[:, 0:n], scalar1=gcol, scalar2=None, op0=ALU.mult),
                             reads=[stk, self.ngk], writes=[obk])
                self.pstore = (dst, ob, obk, n)

        def prologue():
            with ExitStack() as es:
                cst = Caster(es, 0, "pl")
                for p in weight_pieces(0):
                    cst.run(p)
                cst.flush()
                S.barrier()

        def phase_A(l, s, xsrc):
            t0 = s * S_TOK
            with ExitStack() as es:
                Wb = sbt(es, "A_Wb", [128, 8, 6144], BF16)
                Wg = sbt(es, "A_Wg", [128, 8, 256], BF16)
                cw = sbt(es, "A_cw", [128, 8, 4], F32)
                cb = sbt(es, "A_cb", [128, 8], F32)
                gb = sbt(es, "A_gb", [128, 2], F32)
                qg = sbt(es, "A_qg", [128, 2], F32)
                mg = sbt(es, "A_mg", [128, 1024], F32)
                xt = Rot([(sbt(es, "A_xt%d" % i, [128, 1024], F32), "A_xt%d" % i) for i in range(4)])
                hT = Rot([(sbt(es, "A_hT%d" % i, [128, 8, 512], BF16), "A_hT%d" % i) for i in range(2)])
                sqj = sbt(es, "A_sqj", [128, 1024], BF16)
                ssq = Rot([(sbt(es, "A_ssq%d" % i, [128, 2], F32), "A_ssq%d" % i) for i in range(4)])
                xn = Rot([(sbt(es, "A_xn%d" % i, [128, 1024], BF16), "A_xn%d" % i) for i in range(4)])
                cbuf = sbt(es, "A_cbuf", [128, 8, 515], F32)
                acc = Rot([(sbt(es, "A_acc%d" % i, [128, 512], F32), "A_acc%d" % i) for i in range(2)])
                fmo = Rot([(sbt(es, "A_fmo%d" % i, [128, 512], BF16), "A_fmo%d" % i) for i in range(4)])
                tmo = Rot([(sbt(es, "A_tmo%d" % i, [128, 1024], BF16), "A_tmo%d" % i) for i in range(4)])
                tmz = Rot([(sbt(es, "A_tmz%d" % i, [128, 1024], BF16), "A_tmz%d" % i) for i in range(2)])
                gto = Rot([(sbt(es, "A_gto%d" % i, [128, 512], F32), "A_gto%d" % i) for i in range(2)])
                rsb = Rot([(sbt(es, "A_rs%d" % i, [128, 512], F32), "A_rs%d" % i) for i in range(2)])
                sqb = Rot([(sbt(es, "A_sqb%d" % i, [128, 512], BF16), "A_sqb%d" % i) for i in range(2)])
                pmain = Rot([(pst(es, "A_pm%d" % i, [128, 512], F32), "A_pm%d" % i) for i in range(5)])
                pss = Rot([(pst(es, "A_pss%d" % i, [128, 512], F32), "A_pss%d" % i) for i in range(2)])
                ptr = pst(es, "A_ptr", [128, 1024], BF16)
                S.op('pool', lambda e: e.memset(cbuf[:, :, 0:3], 0.0), writes=[("cbuf", c) for c in range(8)])

                def front_load(i):
                    lds = []
                    for j in range(4):
                        xtile, xk = xt.next()
                        S.dma('sp', xtile[:], xsrc[t0 + i * 512 + j * 128:t0 + i * 512 + (j + 1) * 128, :], writes=[xk])
                        lds.append((xtile, xk))
                    return lds

                def front_a(lds):
                    xns = []
                    for j in range(4):
                        xtile, xk = lds[j]
                        sq, sqk = ssq.next()
                        xnt, xnk = xn.next()
                        S.op('act', lambda e: e.activation(out=sqj[:], in_=xtile[:], func=AF.Square, accum_out=sq[:, 0:1]),
                             reads=[xk], writes=["sqj", sqk])
                        S.op('act', lambda e: e.activation(out=sq[:, 1:2], in_=sq[:, 0:1], func=AF.Ln, bias=epsc, scale=1.0 / D),
                             reads=[sqk, "coef"], writes=[sqk])
                        S.op('act', lambda e: e.activation(out=sq[:, 1:2], in_=sq[:, 1:2], func=AF.Exp, scale=-0.5), reads=[sqk], writes=[sqk])
                        S.op('dve', lambda e: e.tensor_scalar(out=xnt[:], in0=xtile[:], scalar1=sq[:, 1:2], scalar2=None, op0=ALU.mult),
                             reads=[xk, sqk], writes=[xnk])
                        xns.append((xnt, xnk))
                    return xns

                def front_b(xns):
                    hTt, hk = hT.next()
                    for j in range(4):
                        xnt, xnk = xns[j]
                        for kc in range(8):
                            S.op('pe', lambda e: e.transpose(ptr[:, kc * 128:(kc + 1) * 128], xnt[:, kc * 128:(kc + 1) * 128], identb[:]),
                                 reads=[xnk, "identb"], writes=["ptr"], signal=(kc == 7))
                        if j % 2:
                            S.op('act', lambda e: e.copy(out=hTt[:, :, j * 128:(j + 1) * 128], in_=ptr[:].rearrange("p (k t) -> p k t", k=8)),
                                 reads=["ptr"], writes=[hk])
                        else:
                            S.op('dve', lambda e: e.tensor_copy(out=hTt[:, :, j * 128:(j + 1) * 128], in_=ptr[:].rearrange("p (k t) -> p k t", k=8)),
                                 reads=["ptr"], writes=[hk])
                    return hTt, hk

                def mm_fm(hTt, hk, wtile, wkeys, col0):
                    if wkeys is None:
                        wkeys = [("Wb", col0 // 512)]
                    ps, pk = pmain.next()
                    for kc in range(8):
                        S.op('pe', lambda e: e.matmul(ps[:], lhsT=wtile[:, kc, col0:col0 + 128], rhs=hTt[:, kc, :], start=(kc == 0), stop=(kc == 7)),
                             reads=[hk] + wkeys, writes=[pk], signal=(kc == 7))
                    return ps, pk

                def mm_tm(hTt, hk, j, col0):
                    ps, pk = pmain.next()
                    for kc in range(8):
                        S.op('pe', lambda e: e.matmul(ps[:], lhsT=hTt[:, kc, j * 128:(j + 1) * 128], rhs=Wb[:, kc, col0:col0 + 512], start=(kc == 0), stop=(kc == 7)),
                             reads=[hk, ("Wb", col0 // 512)], writes=[pk], signal=(kc == 7))
                    return ps, pk

                def tile_groups(i, hTt, hk, mid):
                    tok0 = i * 512
                    deferred = []

                    def run_deferred():
                        while deferred:
                            deferred.pop(0)()

                    def conv_group(c):
                        ps, pk = mm_fm(hTt, hk, Wb, None, c * 128)
                        run_deferred()
                        ck = ("cbuf", c)
                        a, ak = acc.next()
                        S.op('act', lambda e: e.copy(out=cbuf[:, c, 3:515], in_=ps[:]), reads=[pk], writes=[ck])
                        S.op('dve', lambda e: e.tensor_scalar(out=a[:], in0=cbuf[:, c, 0:512], scalar1=cw[:, c, 0:1], scalar2=None, op0=ALU.mult),
                             reads=[ck, "cw"], writes=[ak])
                        for jt in range(1, 4):
                            S.op('dve', lambda e: e.scalar_tensor_tensor(out=a[:], in0=cbuf[:, c, jt:jt + 512], scalar=cw[:, c, jt:jt + 1], in1=a[:],
                                                                         op0=ALU.mult, op1=ALU.add),
                                 reads=[ck, "cw", ak], writes=[ak])
                        fo, fk = fmo.next()
                        S.op('act', lambda e: e.activation(out=fo[:], in_=a[:], func=AF.Silu, bias=cb[:, c:c + 1]), reads=[ak, "cb"], writes=[fk])
                        S.op('pool', lambda e: e.tensor_copy(out=cbuf[:, c, 0:3], in_=cbuf[:, c, 512:515]), reads=[ck], writes=[ck])
                        dst = (s_qT if c < 4 else s_kT)[s, (c % 4) * 128:(c % 4 + 1) * 128, tok0:tok0 + 512]
                        S.dma('sp', dst, fo[:], reads=[fk])

                    def norm_group(c):
                        ps, pk = mm_fm(hTt, hk, Wb, None, 4096 + c * 128)
                        run_deferred()
                        sqt, sqk = sqb.next()
                        S.op('act', lambda e: e.activation(out=sqt[:], in_=ps[:], func=AF.Square), reads=[pk], writes=[sqk])

                        def second():
                            p2, p2k = pss.next()
                            S.op('pe', lambda e: e.matmul(p2[:], lhsT=blk[:], rhs=sqt[:], start=True, stop=True), reads=["blk", sqk], writes=[p2k])
                            rs, rk = rsb.next()
                            S.op('act', lambda e: e.activation(out=rs[:], in_=p2[:], func=AF.Ln, bias=epsc, scale=1.0 / 64), reads=[p2k, "coef"], writes=[rk])
                            S.op('act', lambda e: e.activation(out=rs[:], in_=rs[:], func=AF.Exp, scale=-0.5), reads=[rk], writes=[rk])
                            fo, fk = fmo.next()
                            gcol = qg[:, 0:1] if c < 4 else qg[:, 1:2]
                            S.op('dve', lambda e: e.scalar_tensor_tensor(out=fo[:], in0=ps[:], scalar=gcol, in1=rs[:], op0=ALU.mult, op1=ALU.mult),
                                 reads=[pk, "qg", rk], writes=[fk])
                            dst = (s_aqT if c < 4 else s_akT)[s, (c % 4) * 128:(c % 4 + 1) * 128, tok0:tok0 + 512]
                            S.dma('sp', dst, fo[:], reads=[fk])
                        deferred.append(second)

                    def gate_group(which):
                        ps, pk = mm_fm(hTt, hk, Wg, ["Wg"], which * 128)
                        run_deferred()
                        go, gk = gto.next()
                        S.op('act', lambda e: e.activation(out=go[:], in_=ps[:], func=AF.Identity, bias=gb[:, which:which + 1]), reads=[pk, "gb"], writes=[gk])
                        dst = (s_gI if which == 0 else s_gF)[s, :, tok0:tok0 + 512]
                        S.dma('sp', dst, go[:], reads=[gk])

                    def rows_of(j):
                        return slice(t0 + tok0 + j * 128, t0 + tok0 + (j + 1) * 128)

                    def tm_v(j):
                        to, tk = tmo.next()
                        for b in range(2):
                            ps, pk = mm_tm(hTt, hk, j, 1024 + b * 512)
                            run_deferred()
                            if b == 0:
                                S.op('dve', lambda e: e.tensor_copy(out=to[:, 0:512], in_=ps[:]), reads=[pk], writes=[tk])
                            else:
                                S.op('act', lambda e: e.copy(out=to[:, 512:1024], in_=ps[:]), reads=[pk], writes=[tk])
                        S.dma('sp', s_v[rows_of(j), :], to[:], reads=[tk])

                    def tm_o(j):
                        to, tk = tmo.next()
                        for b in range(2):
                            ps, pk = mm_tm(hTt, hk, j, 2048 + b * 512)
                            run_deferred()
                            S.op('act', lambda e: e.activation(out=to[:, b * 512:(b + 1) * 512], in_=ps[:], func=AF.Sigmoid), reads=[pk], writes=[tk])
                        S.dma('sp', s_o[rows_of(j), :], to[:], reads=[tk])

                    def tm_z(j):
                        to, tk = tmo.next()
                        tz, tzk = tmz.next()
                        for b in range(2):
                            ps, pk = mm_tm(hTt, hk, j, 3072 + b * 512)
                            run_deferred()
                            S.op('act', lambda e: e.activation(out=to[:, b * 512:(b + 1) * 512], in_=ps[:], func=AF.Silu), reads=[pk], writes=[tk])
                        S.op('pool', lambda e: e.tensor_tensor(out=tz[:], in0=to[:], in1=mg[:], op=ALU.mult), reads=[tk, "mg"], writes=[tzk])
                        S.dma('sp', s_z[rows_of(j), :], tz[:], reads=[tzk])

                    def tm_a(j):
                        to, tk = tmo.next()
                        ps, pk = mm_tm(hTt, hk, j, 5120)
                        run_deferred()
                        S.op('dve', lambda e: e.tensor_copy(out=to[:, 0:512], in_=ps[:]), reads=[pk], writes=[tk])
                        ps, pk = mm_tm(hTt, hk, j, 5632)
                        run_deferred()
                        S.op('act', lambda e: e.activation(out=to[:, 512:1024], in_=ps[:], func=AF.Silu), reads=[pk], writes=[tk])
                        S.dma('sp', s_av[rows_of(j), :], to[:, 0:512], reads=[tk])
                        S.dma('sp', s_az[rows_of(j), :], to[:, 512:1024], reads=[tk])

                    gate_group(0)
                    gate_group(1)
                    for c in range(8):
                        norm_group(c)
                    for j in range(4):
                        tm_o(j)
                    if mid is not None:
                        mid()
                    for j in range(4):
                        conv_group(2 * j)
                        tm_z(j)
                        tm_v(j)
                        conv_group(2 * j + 1)
                        tm_a(j)
                    run_deferred()

                ld = {0: front_load(0)}
                S.dma('sp', cw[:], convw[l], writes=["cw"])
                S.dma('sp', cb[:], convb[l], writes=["cb"])
                S.dma('sp', gb[:], gateb[l], writes=["gb"])
                S.dma('sp', qg[:], qkg[l], writes=["qg"])
                S.dma('sp', mg[:], mng[l], writes=["mg"])
                xns0 = front_a(ld[0])
                S.dma('sp', Wg[:].rearrange("p k n -> p (k n)"), ws_g[l], writes=["Wg"])
                wsv = ws_in[l].rearrange("p (k n) -> p k n", k=8)
                for wcb in (8, 9, 4, 5, 0, 1, 6, 7, 2, 3, 10, 11):
                    S.dma('sp', Wb[:, :, wcb * 512:(wcb + 1) * 512], wsv[:, :, wcb * 512:(wcb + 1) * 512], writes=[("Wb", wcb)])
                if NT > 1:
                    ld[1] = front_load(1)
                cur = front_b(xns0)
                for i in range(NT):
                    nxt = {}
                    xns = front_a(ld[i + 1]) if i + 1 < NT else None

                    def mid():
                        if xns is not None:
                            nxt["v"] = front_b(xns)
                        if i + 2 < NT:
                            ld[i + 2] = front_load(i + 2)
                    tile_groups(i, cur[0], cur[1], mid)
                    cur = nxt.get("v")
                S.barrier()

        def phase_B(l, s):
            with ExitStack() as es:
                I = sbt(es, "B_I", [128, S_TOK], F32)
                Fr = sbt(es, "B_F", [128, S_TOK], F32)
                nB = sbt(es, "B_nB", [128, S_TOK], F32)
                M = sbt(es, "B_M", [128, S_TOK], F32)
                ones1 = sbt(es, "B_ones", [128, 1], F32)
                dl = sbt(es, "B_dl", [128, NCH], F32)
                ptb = Rot([(pst(es, "B_pt%d" % i, [128, 512], F32), "B_pt%d" % i) for i in range(2)])
                S.op('pool', lambda e: e.memset(ones1[:], 1.0), writes=["ones1"])
                S.dma('sp', I[:], s_gI[s], writes=["I"])
                S.dma('sp', Fr[:], s_gF[s], writes=["F"])
                S.op('act', lambda e: e.activation(out=Fr[:], in_=Fr[:], func=AF.Exp, scale=-1.0), reads=["F"], writes=["F"])
                S.op('act', lambda e: e.activation(out=Fr[:], in_=Fr[:], func=AF.Ln, bias=1.0), reads=["F"], writes=["F"])
                onesb = ones1[:, 0:1].broadcast_to([128, S_TOK])
                S.op('dve', lambda e: e.tensor_tensor_scan(out=nB[:], data0=onesb, data1=Fr[:], initial=0.0, op0=ALU.mult, op1=ALU.add),
                     reads=["F", "ones1"], writes=["nB"])
                S.op('dve', lambda e: e.tensor_tensor(out=I[:], in0=I[:], in1=nB[:], op=ALU.add), reads=["I", "nB"], writes=["I"])
                S.op('dve', lambda e: e.tensor_tensor_scan(out=M[:], data0=onesb, data1=I[:], initial=0.0, op0=ALU.mult, op1=ALU.max),
                     reads=["I", "ones1"], writes=["M"])
                S.op('dve', lambda e: e.tensor_scalar(out=Fr[:], in0=I[:], scalar1=e0c, scalar2=cstc, op0=ALU.mult, op1=ALU.add),
                     reads=["I", "coef"], writes=["F"])
                S.op('dve', lambda e: e.scalar_tensor_tensor(out=Fr[:], in0=nB[:], scalar=e1c, in1=Fr[:], op0=ALU.mult, op1=ALU.add),
                     reads=["nB", "coef", "F"], writes=["F"])
                Mv = M[:].rearrange("p (c j) -> p c j", j=128)
                Mlast_b = Mv[:, :, 127:128].broadcast_to([128, NCH, 128])
                S.op('dve', lambda e: e.tensor_tensor(out=nB[:].rearrange("p (c j) -> p c j", j=128), in0=Fr[:].rearrange("p (c j) -> p c j", j=128),
                                                      in1=Mlast_b, op=ALU.subtract), reads=["F", "M"], writes=["nB"])
                S.op('act', lambda e: e.activation(out=I[:], in_=nB[:], func=AF.Exp), reads=["nB"], writes=["I"])
                for c4 in range(NCH // 4):
                    pt, ptk = ptb.next()
                    for k in range(4):
                        c = c4 * 4 + k
                        S.op('pe', lambda e: e.transpose(pt[:, k * 128:(k + 1) * 128], I[:, c * 128:(c + 1) * 128], identf[:]),
                             reads=["I", "identf"], writes=[ptk], signal=(k == 3))
                    S.op('dve', lambda e: e.tensor_copy(out=colz[:, c4 * 4:(c4 + 1) * 4, :, :],
                                                        in_=pt[:].rearrange("p (k h r) -> p k h r", k=4, h=4)[:, :, :, 0:2]),
                         reads=[ptk], writes=["colz"])
                Ml = M[:, 127:S_TOK:128]
                S.op('dve', lambda e: e.tensor_scalar(out=dl[:, 0:1], in0=M[:, 127:128], scalar1=-1.0, scalar2=None, op0=ALU.mult), reads=["M"], writes=["dl"])
                if NCH > 1:
                    S.op('dve', lambda e: e.tensor_tensor(out=dl[:, 1:NCH], in0=M[:, 127:S_TOK - 128:128], in1=M[:, 255:S_TOK:128], op=ALU.subtract),
                         reads=["M"], writes=["dl"])
                S.op('act', lambda e: e.activation(out=dl[:], in_=dl[:], func=AF.Exp), reads=["dl"], writes=["dl"])
                S.dma('sp', s_dec[:, :], dl[:], reads=["dl"], writes=["s_dec"])
                for h in range(4):
                    S.dma('sp', decb[:, h, :], s_dec[32 * h:32 * h + 1, :].broadcast_to([128, NCH]), reads=["s_dec"], writes=["decb"])
                if debug:
                    S.dma('sp', dbg["colz"][:, :], colz[:].rearrange("p c h r -> p (c h r)"), reads=["colz"])
                    S.dma('sp', dbg["decb"][:, :], decb[:].rearrange("p h c -> p (h c)"), reads=["decb"])
                S.barrier()

        def phase_C(l, s):
            t0 = s * S_TOK
            NG = NCH // 4
            with ExitStack() as es:
                qTg = Rot([(sbt(es, "C_q%d" % i, [128, 4, 512], BF16), "C_q%d" % i) for i in range(2)])
                kTg = Rot([(sbt(es, "C_k%d" % i, [128, 4, 512], BF16), "C_k%d" % i) for i in range(2)])
                vg = Rot([(sbt(es, "C_v%d" % i, [128, 4, 4, 258], BF16), "C_v%d" % i) for i in range(2)])
                og = Rot([(sbt(es, "C_o%d" % i, [128, 4, 1024], BF16), "C_o%d" % i) for i in range(2)])
                zg = Rot([(sbt(es, "C_z%d" % i, [128, 4, 1024], BF16), "C_z%d" % i) for i in range(2)])
                hmg = Rot([(sbt(es, "C_hm%d" % i, [128, 4, 1024], BF16), "C_hm%d" % i) for i in range(2)])
                Cm = sbt(es, "C_Cm", [128, 4, 257], F32)
                Cb = sbt(es, "C_Cb", [128, 4, 258], BF16)
                STt = Rot([(sbt(es, "C_ST%d" % i, [128, 4, 128], BF16), "C_ST%d" % i) for i in range(2)])
                kwt = Rot([(sbt(es, "C_kw%d" % i, [128, 4, 128], BF16), "C_kw%d" % i) for i in range(2)])
                t1t = Rot([(sbt(es, "C_t1%d" % i, [128, 4, 256], F32), "C_t1%d" % i) for i in range(2)])
                sqj = sbt(es, "C_sqj", [128, 256], BF16)
                sm = Rot([(sbt(es, "C_sm%d" % i, [128, 16], F32), "C_sm%d" % i) for i in range(3)])
                ps_s = pst(es, "C_pss", [128, 512], F32)
                ps_k = pst(es, "C_psk", [128, 1024], BF16)
                ps_o = pst(es, "C_pso", [128, 4, 512], F32)
                ps_c = Rot([(pst(es, "C_psc%d" % i, [128, 512], F32), "C_psc%d" % i) for i in range(2)])
                S.op('pool', lambda e: e.memset(Cm[:], 0.0), writes=[("Cm", h) for h in range(4)])
                for (vt, vk) in vg.items:
                    S.op('pool', lambda e: e.memset(vt[:], 1.0), writes=[vk])

                def load_group(g):
                    q, qk_ = qTg.next()
                    k, kk_ = kTg.next()
                    v, vk = vg.next()
                    o, ok = og.next()
                    z, zk = zg.next()
                    tk0 = g * 512
                    S.dma('sp', q[:], s_qT[s, :, tk0:tk0 + 512].rearrange("(h p) t -> p h t", p=128), writes=[qk_])
                    S.dma('sp', k[:], s_kT[s, :, tk0:tk0 + 512].rearrange("(h p) t -> p h t", p=128), writes=[kk_])
                    rows = slice(t0 + tk0, t0 + tk0 + 512)
                    for cc in range(4):
                        r4 = slice(t0 + tk0 + cc * 128, t0 + tk0 + (cc + 1) * 128)
                        S.dma('sp', v[:, cc, :, 0:256], s_v[r4, :].rearrange("p (h e) -> p h e", h=4), writes=[vk])
                    S.dma('sp', o[:], s_o[rows, :].rearrange("(c p) n -> p c n", p=128), writes=[ok])
                    S.dma('sp', z[:], s_z[rows, :].rearrange("(c p) n -> p c n", p=128), writes=[zk])
                    return (q, qk_, k, kk_, v, vk, o, ok, z, zk)

                pend = []

                def stage5():
                    (t1, t1k, smt, smk, z, zk, cc, hm, hmk, g, last) = pend.pop(0)
                    for h in range(4):
                        S.op('dve', lambda e: e.scalar_tensor_tensor(out=hm[:, cc, h * 256:(h + 1) * 256], in0=t1[:, h, :], scalar=smt[:, 12 + h:13 + h],
                                                                     in1=z[:, cc, h * 256:(h + 1) * 256], op0=ALU.mult, op1=ALU.mult),
                             reads=[(t1k, h), smk, zk], writes=[hmk])
                    if last:
                        rows = slice(t0 + g * 512, t0 + (g + 1) * 512)
                        S.dma('sp', s_hm[rows, :].rearrange("(c p) n -> p c n", p=128), hm[:], reads=[hmk])

                nxt = load_group(0)
                for g in range(NG):
                    (q, qk_, k, kk_, v, vk, o, ok, z, zk) = nxt
                    hm, hmk = hmg.next()
                    for cc in range(4):
                        c = g * 4 + cc
                        ts = slice(cc * 128, (cc + 1) * 128)
                        st, stk = STt.next()
                        kw, kwk = kwt.next()
                        t1, t1k = t1t.next()
                        smt, smk = sm.next()
                        for h in range(4):
                            S.op('pe', lambda e: e.matmul(ps_s[:, h * 128:(h + 1) * 128], lhsT=k[:, h, ts], rhs=q[:, h, ts], start=True, stop=True),
                                 reads=[kk_, qk_], writes=["C_pss"], signal=(h == 3))
                        for h in range(4):
                            S.op('pe', lambda e: e.transpose(ps_k[:, h * 128:(h + 1) * 128], k[:, h, ts], identb[:]), reads=[kk_, "identb"], writes=["C_psk"],
                                 signal=(h == 3))
                        for h in range(4):
                            S.op('dve', lambda e: e.scalar_tensor_tensor(out=st[:, h, :], in0=ps_s[:, h * 128:(h + 1) * 128], scalar=colz[:, c, h, 0:1],
                                                                         in1=maskt[:, 0:128], op0=ALU.mult, op1=ALU.mult),
                                 reads=["C_pss", "colz", "maskt"], writes=[(stk, h)])
                        for h in range(4):
                            S.op('act', lambda e: e.activation(out=kw[:, h, :], in_=ps_k[:, h * 128:(h + 1) * 128], func=AF.Copy, scale=colz[:, c, h, 0:1]),
                                 reads=["C_psk", "colz"], writes=[(kwk, h)])
                        for h in range(4):
                            S.op('act', lambda e: e.activation(out=Cb[:, h, 0:257], in_=Cm[:, h, :], func=AF.Copy, scale=decb[:, h, c:c + 1]),
                                 reads=[("Cm", h), "decb"], writes=[("Cb", h)])
                        if pend:
                            stage5()
                        if cc == 0 and g + 1 < NG:
                            nxt = load_group(g + 1)
                        pcs = []
                        for h in range(4):
                            vext = v[:, cc, h, 0:257]
                            S.op('pe', lambda e: e.matmul(ps_o[:, h, 0:257], lhsT=st[:, h, :], rhs=vext, start=True, stop=False), reads=[(stk, h), vk],
                                 writes=[("C_pso", h)], signal=False)
                            S.op('pe', lambda e: e.matmul(ps_o[:, h, 0:257], lhsT=q[:, h, ts], rhs=Cb[:, h, 0:257], start=False, stop=True),
                                 reads=[qk_, ("Cb", h)], writes=[("C_pso", h)])
                            psc, psck = ps_c.next()
                            S.op('pe', lambda e: e.matmul(psc[:, 0:257], lhsT=kw[:, h, :], rhs=vext, start=True, stop=True), reads=[(kwk, h), vk], writes=[psck])
                            S.op('dve', lambda e: e.scalar_tensor_tensor(out=Cm[:, h, :], in0=Cm[:, h, :], scalar=decb[:, h, c:c + 1], in1=psc[:, 0:257],
                                                                         op0=ALU.mult, op1=ALU.add),
                                 reads=[("Cm", h), "decb", psck], writes=[("Cm", h)])
                        pso_keys = [("C_pso", h) for h in range(4)]
                        denv = ps_o[:, :, 256:257]
                        S.op('dve', lambda e: e.tensor_scalar(out=smt[:, 0:4].rearrange("p (h o) -> p h o", o=1), in0=denv, scalar1=-1.0, scalar2=None, op0=ALU.mult),
                             reads=pso_keys, writes=[smk])
                        S.op('dve', lambda e: e.tensor_tensor(out=smt[:, 0:4], in0=smt[:, 0:4], in1=colz[:, c, :, 1], op=ALU.max), reads=[smk, "colz"], writes=[smk])
                        S.op('dve', lambda e: e.tensor_tensor(out=smt[:, 0:4].rearrange("p (h o) -> p h o", o=1), in0=denv,
                                                              in1=smt[:, 0:4].rearrange("p (h o) -> p h o", o=1), op=ALU.max), reads=pso_keys + [smk], writes=[smk])
                        S.op('dve', lambda e: e.reciprocal(out=smt[:, 4:8], in_=smt[:, 0:4]), reads=[smk], writes=[smk])
                        for h in range(4):
                            S.op('dve', lambda e: e.scalar_tensor_tensor(out=t1[:, h, :], in0=ps_o[:, h, 0:256], scalar=smt[:, 4 + h:5 + h],
                                                                         in1=o[:, cc, h * 256:(h + 1) * 256], op0=ALU.mult, op1=ALU.mult),
                                 reads=[("C_pso", h), smk, ok], writes=[(t1k, h)])
                        for h in range(4):
                            S.op('act', lambda e: e.activation(out=sqj[:], in_=t1[:, h, :], func=AF.Square, accum_out=smt[:, 8 + h:9 + h]),
                                 reads=[(t1k, h)], writes=["C_sqj", smk])
                        S.op('act', lambda e: e.activation(out=smt[:, 12:16], in_=smt[:, 8:12], func=AF.Ln, bias=epsc, scale=1.0 / 256), reads=[smk, "coef"], writes=[smk])
                        S.op('act', lambda e: e.activation(out=smt[:, 12:16], in_=smt[:, 12:16], func=AF.Exp, scale=-0.5), reads=[smk], writes=[smk])
                        pend.append((t1, t1k, smt, smk, z, zk, cc, hm, hmk, g, cc == 3))
                while pend:
                    stage5()
                S.barrier()

        def phase_D(l, s):
            t0 = s * S_TOK
            with ExitStack() as es:
                QT = [sbt(es, "D_Q%d" % j, [128, 2, S_TOK], BF16) for j in range(4)]
                KT = [sbt(es, "D_K%d" % j, [128, S_TOK], BF16) for j in range(4)]
                NB = S_TOK // 128
                Vd = Rot([(sbt(es, "D_V%d" % i, [128, NB, 8, 66], BF16), "D_V%d" % i) for i in range(2)])
                Vstg = Rot([(sbt(es, "D_Vstg%d" % i, [128, 8, 512], BF16), "D_Vstg%d" % i) for i in range(2)])
                pex = Rot([(sbt(es, "D_pe%d" % i, [128, 512], BF16), "D_pe%d" % i) for i in range(4)])
                pmk = Rot([(sbt(es, "D_pm%d" % i, [128, 512], BF16), "D_pm%d" % i) for i in range(4)])
                ostg = Rot([(sbt(es, "D_os%d" % i, [128, 8, 65], F32), "D_os%d" % i) for i in range(3)])
                ps_s = Rot([(pst(es, "D_pss%d" % i, [128, 512], F32), "D_pss%d" % i) for i in range(4)])
                ps_o = Rot([(pst(es, "D_pso%d" % i, [128, 512], F32), "D_pso%d" % i) for i in range(4)])
                mask4 = maskt[:, 256:768]
                for j in range(4):
                    S.op('dve', lambda e: e.memset(QT[j][64:128, 0, :], 0.0), writes=[("QT", j, "z0")])
                    S.op('pool', lambda e: e.memset(QT[j][0:64, 1, :], 0.0), writes=[("QT", j, "z1")])
                    S.dma('sp', QT[j][0:64, 0, :], s_aqT[s, j * 128:j * 128 + 64, :], writes=[("QT", j, "a")])
                    S.dma('sp', QT[j][64:128, 1, :], s_aqT[s, j * 128 + 64:(j + 1) * 128, :], writes=[("QT", j, "b")])
                    S.dma('sp', KT[j][:], s_akT[s, j * 128:(j + 1) * 128, :], writes=[("KT", j)])
                for (vt, vk) in Vd.items:
                    S.op('pool', lambda e: e.memset(vt[:, :, :, 64:66], 1.0), writes=[(vk, "ones")])
                av_seq = s_av[t0:t0 + S_TOK, :]
                vcnt = [0]

                def load_V(d):
                    nbd_ = S_TOK // d // 128
                    V_, Vk_ = Vd.next()
                    srcv = av_seq.rearrange("(nb i dd) n -> dd i nb n", dd=d, i=128)
                    for b0 in range(0, NB, 8):
                        vs, vsk = Vstg.next()
                        if nbd_ >= 8:
                            r_, n0 = b0 // nbd_, b0 % nbd_
                            S.dma('sp', vs[:], srcv[r_][:, n0:n0 + 8, :], writes=[vsk])
                        else:
                            for r_ in range(b0 // nbd_, (b0 + 8) // nbd_):
                                o_ = r_ * nbd_ - b0
                                S.dma('sp', vs[:, o_:o_ + nbd_, :], srcv[r_], writes=[vsk])
                        eng = 'dve' if vcnt[0] % 2 == 0 else 'act'
                        vcnt[0] += 1
                        if eng == 'dve':
                            S.op('dve', lambda e: e.tensor_copy(out=V_[:, b0:b0 + 8, :, 0:64], in_=vs[:].rearrange("p b (h e) -> p b h e", h=8)),
                                 reads=[vsk], writes=[(Vk_, b0)])
                        else:
                            S.op('act', lambda e: e.copy(out=V_[:, b0:b0 + 8, :, 0:64], in_=vs[:].rearrange("p b (h e) -> p b h e", h=8)),
                                 reads=[vsk], writes=[(Vk_, b0)])
                    return V_, Vk_

                nextV = load_V(PATTERNS[0])
                for pi, d in enumerate(PATTERNS):
                    nbd = S_TOK // d // 128
                    V, Vk = nextV
                    od_seq = s_od[pi, t0:t0 + S_TOK, :].rearrange("(nb i dd) n -> dd nb i n", dd=d, i=128)
                    units = []
                    for r in range(d):
                        for nb in range(nbd):
                            for p in range(4):
                                units.append((r, nb, p))
                    pending = []
                    state = {}

                    def tok(r, nb):
                        st_ = r + d * 128 * nb
                        return slice(st_, st_ + d * 127 + 1, d)

                    def emit_qk(u):
                        r, nb, p = u
                        qkeys = [("QT", p, x) for x in ("z0", "z1", "a", "b")] + [("KT", p)]
                        pss, pssk = ps_s.next()
                        W = 512 if nb > 0 else 256
                        S.op('pe', lambda e: e.matmul(pss[:, 0:256].rearrange("p (a i) -> p a i", a=2), lhsT=KT[p][:, tok(r, nb)], rhs=QT[p][:, :, tok(r, nb)],
                                                      start=True, stop=True), reads=qkeys, writes=[pssk], signal=(nb == 0))
                        if nb > 0:
                            S.op('pe', lambda e: e.matmul(pss[:, 256:512].rearrange("p (a i) -> p a i", a=2), lhsT=KT[p][:, tok(r, nb - 1)], rhs=QT[p][:, :, tok(r, nb)],
                                                          start=True, stop=True), reads=qkeys, writes=[pssk])
                        px, pxk = pex.next()
                        S.op('act', lambda e: e.activation(out=px[:, 0:W], in_=pss[:, 0:W], func=AF.Exp, scale=0.125), reads=[pssk], writes=[pxk])
                        pm, pmk_ = pmk.next()
                        S.op('dve', lambda e: e.tensor_tensor(out=pm[:, 0:W], in0=px[:, 0:W], in1=mask4[:, 0:W], op=ALU.mult), reads=[pxk, "maskt"], writes=[pmk_])
                        pending.append((u, pm, pmk_))

                    def emit_pv():
                        (u, pm, pmk_) = pending.pop(0)
                        r, nb, p = u
                        b = r * nbd + nb
                        if p % 2 == 0:
                            state["po"] = ps_o.next()
                        po, pok = state["po"]
                        for a in range(2):
                            h = 2 * p + a
                            hs = (h % 4) * 65
                            last = (a == 1)
                            S.op('pe', lambda e: e.matmul(po[:, hs:hs + 65], lhsT=pm[:, a * 128:(a + 1) * 128], rhs=V[:, b, h, 0:65], start=True, stop=(nb == 0)),
                                 reads=[pmk_, (Vk, (b // 8) * 8), (Vk, "ones")], writes=[pok], signal=(nb == 0 and last))
                            if nb > 0:
                                S.op('pe', lambda e: e.matmul(po[:, hs:hs + 65], lhsT=pm[:, 256 + a * 128:256 + (a + 1) * 128], rhs=V[:, b - 1, h, 0:65],
                                                              start=False, stop=True),
                                     reads=[pmk_, (Vk, ((b - 1) // 8) * 8), (Vk, "ones")], writes=[pok], signal=last)
                        if p % 2 == 1:
                            if p == 1:
                                state["os"] = ostg.next()
                            os_, osk = state["os"]
                            if p == 1:
                                S.op('dve', lambda e: e.tensor_copy(out=os_[:, 0:4, :], in_=po[:, 0:260].rearrange("p (h e) -> p h e", h=4)), reads=[pok], writes=[(osk, 0)])
                            else:
                                S.op('act', lambda e: e.copy(out=os_[:, 4:8, :], in_=po[:, 0:260].rearrange("p (h e) -> p h e", h=4)), reads=[pok], writes=[(osk, 1)])
                                S.dma('sp', od_seq[r, nb], os_[:].rearrange("p h e -> p (h e)"), reads=[(osk, 0), (osk, 1)])

                    LAG = 3
                    for ui, u in enumerate(units):
                        emit_qk(u)
                        if len(pending) > LAG:
                            emit_pv()
                        if ui == len(units) // 2 and pi + 1 < len(PATTERNS):
                            nextV = load_V(PATTERNS[pi + 1])
                    while pending:
                        emit_pv()
                S.barrier()

        def phase_E(l, s, xsrc, xdst):
            t0 = s * S_TOK
            with ExitStack() as es:
                Wo = sbt(es, "E_Wo", [128, 12, 1024], BF16)
                S.dma('sp', Wo[:].rearrange("p m n -> p (m n)"), ws_out[l], writes=["Wo"])
                xt = Rot([(sbt(es, "E_xt%d" % i, [128, 1024], F32), "E_xt%d" % i) for i in range(2)])
                hmt = Rot([(sbt(es, "E_hm%d" % i, [128, 1024], BF16), "E_hm%d" % i) for i in range(2)])
                o3 = Rot([(sbt(es, "E_o3%d" % i, [128, 3, 520], F32), "E_o3%d" % i) for i in range(2)])
                azt = Rot([(sbt(es, "E_az%d" % i, [128, 512], BF16), "E_az%d" % i) for i in range(2)])
                osum = Rot([(sbt(es, "E_os%d" % i, [128, 520], F32), "E_os%d" % i) for i in range(2)])
                rden = Rot([(sbt(es, "E_rd%d" % i, [128, 8], F32), "E_rd%d" % i) for i in range(2)])
                ha = Rot([(sbt(es, "E_ha%d" % i, [128, 512], F32), "E_ha%d" % i) for i in range(2)])
                ga = Rot([(sbt(es, "E_ga%d" % i, [128, 512], BF16), "E_ga%d" % i) for i in range(2)])
                gT = Rot([(sbt(es, "E_gT%d" % i, [128, 12, 128], BF16), "E_gT%d" % i) for i in range(2)])
                xo = Rot([(sbt(es, "E_xo%d" % i, [128, 1024], F32), "E_xo%d" % i) for i in range(2)])
                ptr = Rot([(pst(es, "E_ptr%d" % i, [128, 1024], BF16), "E_ptr%d" % i) for i in range(2)])
                ptr2 = Rot([(pst(es, "E_pt2%d" % i, [128, 1024], BF16), "E_pt2%d" % i) for i in range(2)])
                py = Rot([(pst(es, "E_py%d" % i, [128, 512], F32), "E_py%d" % i) for i in range(4)])
                NTL = S_TOK // 128
                jobs = []
                if l + 1 < DEPTH:
                    allp = weight_pieces(l + 1)
                    per = (len(allp) + NSEQ - 1) // NSEQ
                    jobs = allp[s * per:(s + 1) * per]
                    cst = Caster(es, l + 1, "E_pl")

                def load(i):
                    rows = slice(t0 + i * 128, t0 + (i + 1) * 128)
                    a = xt.next(); b = hmt.next(); c = o3.next(); dd = azt.next()
                    S.dma('sp', a[0][:], xsrc[rows, :], writes=[a[1]])
                    S.dma('sp', b[0][:], s_hm[rows, :], writes=[b[1]])
                    S.dma('sp', c[0][:], s_od[:, rows, :].rearrange("g p n -> p g n"), writes=[c[1]])
                    S.dma('sp', dd[0][:], s_az[rows, :], writes=[dd[1]])
                    return a, b, c, dd

                def prep(ld_):
                    (xtt, xtk), (hmtt, hmk), (o3t, o3k), (azz, azk) = ld_
                    os_, osk = osum.next()
                    S.op('dve', lambda e: e.tensor_tensor(out=os_[:], in0=o3t[:, 0, :], in1=o3t[:, 1, :], op=ALU.add), reads=[o3k], writes=[osk])
                    S.op('dve', lambda e: e.tensor_tensor(out=os_[:], in0=os_[:], in1=o3t[:, 2, :], op=ALU.add), reads=[o3k, osk], writes=[osk])
                    osv = os_[:].rearrange("p (h e) -> p h e", h=8)
                    rd, rdk = rden.next()
                    S.op('dve', lambda e: e.reciprocal(out=rd[:].rearrange("p (h o) -> p h o", o=1), in_=osv[:, :, 64:65]), reads=[osk], writes=[rdk])
                    hat, hak = ha.next()
                    S.op('dve', lambda e: e.tensor_tensor(out=hat[:].rearrange("p (h e) -> p h e", h=8), in0=osv[:, :, 0:64],
                                                          in1=rd[:].rearrange("p (h o) -> p h o", o=1).broadcast_to([128, 8, 64]), op=ALU.mult),
                         reads=[osk, rdk], writes=[hak])
                    gat, gak = ga.next()
                    S.op('pool', lambda e: e.tensor_tensor(out=gat[:], in0=hat[:], in1=azz[:], op=ALU.mult), reads=[hak, azk], writes=[gak])
                    gTt, gTk = gT.next()
                    p1, p1k = ptr.next()
                    for mc in range(8):
                        S.op('pe', lambda e: e.transpose(p1[:, mc * 128:(mc + 1) * 128], hmtt[:, mc * 128:(mc + 1) * 128], identb[:]),
                             reads=[hmk, "identb"], writes=[p1k], signal=(mc == 7))
                    S.op('act', lambda e: e.copy(out=gTt[:, 0:8, :], in_=p1[:].rearrange("p (m t) -> p m t", m=8)), reads=[p1k], writes=[(gTk, 0)])
                    p2, p2k = ptr2.next()
                    for mc in range(4):
                        S.op('pe', lambda e: e.transpose(p2[:, mc * 128:(mc + 1) * 128], gat[:, mc * 128:(mc + 1) * 128], identb[:]),
                             reads=[gak, "identb"], writes=[p2k], signal=(mc == 3))
                    S.op('act', lambda e: e.copy(out=gTt[:, 8:12, :], in_=p2[:, 0:512].rearrange("p (m t) -> p m t", m=4)), reads=[p2k], writes=[(gTk, 1)])
                    return (gTt, gTk, xtt, xtk)

                def project(i, pr):
                    (gTt, gTk, xtt, xtk) = pr
                    rows = slice(t0 + i * 128, t0 + (i + 1) * 128)
                    xot, xok = xo.next()
                    for half in range(2):
                        pyt, pyk = py.next()
                        for mc in range(12):
                            S.op('pe', lambda e: e.matmul(pyt[:], lhsT=gTt[:, mc, :], rhs=Wo[:, mc, half * 512:(half + 1) * 512], start=(mc == 0), stop=(mc == 11)),
                                 reads=[(gTk, 0), (gTk, 1), "Wo"], writes=[pyk], signal=(mc == 11))
                        S.op('dve', lambda e: e.tensor_tensor(out=xot[:, half * 512:(half + 1) * 512], in0=xtt[:, half * 512:(half + 1) * 512], in1=pyt[:], op=ALU.add),
                             reads=[xtk, pyk], writes=[(xok, half)])
                    S.dma('sp', xdst[rows, :], xot[:], reads=[(xok, 0), (xok, 1)])

                lds = {0: load(0)}
                if NTL > 1:
                    lds[1] = load(1)
                prs = {0: prep(lds[0])}
                for i in range(NTL):
                    if i + 1 < NTL:
                        prs[i + 1] = prep(lds[i + 1])
                    project(i, prs.pop(i))
                    if i + 2 < NTL:
                        lds[i + 2] = load(i + 2)
                    for _ in range(2):
                        if jobs and (i % 2 == 1 or len(jobs) > NTL - i):
                            cst.run(jobs.pop(0))
                while jobs:
                    cst.run(jobs.pop(0))
                if l + 1 < DEPTH:
                    cst.flush()
                S.barrier()

        prologue()
        for l in range(DEPTH):
            xsrc = x_in if l == 0 else xs[(l - 1) % 2]
            xdst = y_out if l == DEPTH - 1 else xs[l % 2]
            for s in range(NSEQ):
                phase_A(l, s, xsrc)
                phase_B(l, s)
                phase_C(l, s)
                phase_D(l, s)
                phase_E(l, s, xsrc, xdst)
        S.barrier()
        stats = dict(n_ins=S.n_ins, n_wait=S.n_wait, cnt=dict(S.cnt))
    return nc, stats


def host_constants():
    k = np.arange(128)[:, None]
    q = np.arange(128)[None, :]
    mask = np.concatenate([(k <= q), (k >= q), (k <= q), (k <= q), (k >= q), (k >= q)], axis=1).astype(np.float32).astype(NPBF)
    blk = (np.arange(128)[:, None] // 64 == np.arange(128)[None, :] // 64).astype(np.float32).astype(NPBF)
    coef = np.zeros((128, 4), np.float32)
    coef[0::32, 0] = 1.0
    coef[1::32, 1] = 1.0
    coef[0::32, 2] = -0.5 * np.log(128.0)
    coef[:, 3] = EPS
    return {"c_identb": np.eye(128, dtype=np.float32).astype(NPBF), "c_identf": np.eye(128, dtype=np.float32),
            "c_mask": mask, "c_blk": blk, "c_coef": coef}


def layout_params(norm_g, w_in, gate_b, conv_w, conv_b, m_norm_g, q_norm_g, k_norm_g):
    DEPTH = norm_g.shape[0]
    normg = np.ascontiguousarray(norm_g.reshape(DEPTH, 8, 128).transpose(0, 2, 1))
    convw = np.ascontiguousarray(conv_w.reshape(DEPTH, 4, 8, 128).transpose(0, 3, 2, 1))
    convb = np.ascontiguousarray(conv_b.reshape(DEPTH, 8, 128).transpose(0, 2, 1))
    gateb = np.zeros((DEPTH, 128, 2), np.float32)
    w_g = np.zeros((DEPTH, D, 256), np.float32)
    for h in range(4):
        for r in range(3):
            gateb[:, 32 * h + r, 0] = gate_b[:, h]
            gateb[:, 32 * h + r, 1] = gate_b[:, 4 + h]
            w_g[:, :, 32 * h + r] = w_in[:, :, 4096 + h]
            w_g[:, :, 128 + 32 * h + r] = w_in[:, :, 4100 + h]
    qkg = np.stack([np.tile(q_norm_g, (1, 2)), np.tile(k_norm_g, (1, 2))], axis=-1).astype(np.float32)
    mng = np.ascontiguousarray(np.broadcast_to(m_norm_g[:, None, :], (DEPTH, 128, 1024))).astype(np.float32)
    return dict(normg=normg, convw=convw, convb=convb, gateb=gateb, w_g=w_g, qkg=np.ascontiguousarray(qkg), mng=mng)


_CACHE = {}


def run(x, norm_g, w_in, gate_b, conv_w, conv_b, m_norm_g, q_norm_g, k_norm_g, w_out, n_cores, debug=False):
    B, S_TOK, _ = x.shape
    DEPTH = norm_g.shape[0]
    NSEQ = B // n_cores
    key = (S_TOK, NSEQ, DEPTH, debug)
    if key not in _CACHE:
        _CACHE[key] = build_program(S_TOK, NSEQ, DEPTH, debug)
    nc, stats = _CACHE[key]
    shared = dict(host_constants())
    shared.update(layout_params(norm_g, w_in, gate_b, conv_w, conv_b, m_norm_g, q_norm_g, k_norm_g))
    shared["w_in"] = np.ascontiguousarray(w_in, dtype=np.float32)
    shared["w_out"] = np.ascontiguousarray(w_out, dtype=np.float32)
    in_maps = []
    for c in range(n_cores):
        m = dict(shared)
        m["x"] = np.ascontiguousarray(x[c * NSEQ:(c + 1) * NSEQ].reshape(NSEQ * S_TOK, D), dtype=np.float32)
        in_maps.append(m)
    res = run_bass_kernel_spmd(nc, in_maps, core_ids=list(range(n_cores)))
    outs = [np.asarray(r["y"]).reshape(NSEQ, S_TOK, D) for r in res.results]
    return np.concatenate(outs, axis=0).astype(np.float32), res, stats


def kernel(x, norm_g, w_in, gate_b, conv_w, conv_b, m_norm_g, q_norm_g, k_norm_g, w_out):
    args = [np.asarray(a) for a in (x, norm_g, w_in, gate_b, conv_w, conv_b, m_norm_g, q_norm_g, k_norm_g, w_out)]
    out, _, _ = run(*args, n_cores=8)
    return out
```

```python
import numpy as np
import ml_dtypes
from contextlib import ExitStack
import concourse.bass as bass
import concourse.mybir as mybir
from concourse.bass_utils import run_bass_kernel_spmd

F32 = mybir.dt.float32
BF16 = mybir.dt.bfloat16
AF = mybir.ActivationFunctionType
ALU = mybir.AluOpType
AX = mybir.AxisListType
NPBF = ml_dtypes.bfloat16

D = 1024
NIN = 6152
DMIX = 1536
EPS = 1e-6
PATTERNS = (1, 4, 16)


class Sched:
    NDMA = 48

    def __init__(self, nc, es):
        self.nc = nc
        self.engs = {'pe': nc.tensor, 'act': nc.scalar, 'dve': nc.vector, 'pool': nc.gpsimd, 'sp': nc.sync}
        self.sems = {e: es.enter_context(nc.semaphore("sem_" + e)) for e in self.engs}
        self.cnt = {e: 0 for e in self.engs}
        self.known = {e: {} for e in self.engs}
        self.dma_sems = [es.enter_context(nc.semaphore("dsem%d" % i)) for i in range(self.NDMA)]
        self.dma_cnt = [0] * self.NDMA
        self.dma_rr = 0
        self.state = {}
        self.semobj = {}
        for e, s in self.sems.items():
            self.semobj["sem_" + e] = s
        for i, s in enumerate(self.dma_sems):
            self.semobj["dsem%d" % i] = s
        self.n_wait = 0
        self.n_ins = 0
        self.attach = True

    def _need(self, eng, tickets):
        kn = self.known[eng]
        own = "sem_" + eng
        best = {}
        for (sn, v) in tickets:
            if sn == own and (eng == 'pe' or v > self.cnt[eng]):
                continue
            if kn.get(sn, 0) >= v:
                continue
            if best.get(sn, 0) < v:
                best[sn] = v
        items = list(best.items())
        attach = None
        if self.attach and items:
            attach = items.pop()
            kn[attach[0]] = attach[1]
        for sn, v in items:
            self.engs[eng].wait_ge(self.semobj[sn], v)
            kn[sn] = v
            self.n_wait += 1
        return attach

    def _collect(self, eng, reads, writes):
        own = "sem_" + eng
        t = []
        for k in reads:
            st = self.state.get(k)
            if st:
                t.extend(st[0])
        for k in writes:
            st = self.state.get(k)
            if st:
                t.extend(st[0])
                t.extend(x for x in st[1] if x[0] != own)
        return t

    def _update(self, ticket, reads, writes):
        for k in reads:
            st = self.state.setdefault(k, ([], []))
            st[1].append(ticket)
            if len(st[1]) > 4096:
                best = {}
                for (sn, v) in st[1]:
                    if best.get(sn, 0) < v:
                        best[sn] = v
                st[1][:] = list(best.items())
        for k in writes:
            self.state[k] = ([ticket], [])

    def op(self, eng, fn, reads=(), writes=(), signal=True):
        att = self._need(eng, self._collect(eng, reads, writes))
        ins = fn(self.engs[eng])
        if att is not None:
            ins._wait_ge(self.semobj[att[0]], att[1])
        self.n_ins += 1
        sn = "sem_" + eng
        if signal:
            self.cnt[eng] += 1
            ins.then_inc(self.sems[eng], 1)
            ticket = (sn, self.cnt[eng])
        else:
            ticket = (sn, self.cnt[eng] + 1)
        self._update(ticket, reads, writes)
        return ticket

    def dma(self, eng, out, in_, reads=(), writes=(), **kw):
        i = self.dma_rr
        self.dma_rr = (self.dma_rr + 1) % self.NDMA
        sn = "dsem%d" % i
        tickets = self._collect(eng, reads, writes)
        if self.dma_cnt[i] > 0:
            tickets.append((sn, self.dma_cnt[i]))
        att = self._need(eng, tickets)
        ins = self.engs[eng].dma_start(out=out, in_=in_, **kw)
        if att is not None:
            ins._wait_ge(self.semobj[att[0]], att[1])
        self.n_ins += 1
        self.dma_cnt[i] += 16
        ins.then_inc(self.dma_sems[i], 16)
        ticket = (sn, self.dma_cnt[i])
        self._update(ticket, reads, writes)
        return ticket

    def barrier(self):
        allt = [("sem_" + e, self.cnt[e]) for e in self.engs if self.cnt[e] > 0]
        allt += [("dsem%d" % i, c) for i, c in enumerate(self.dma_cnt) if c > 0]
        sv = self.attach
        self.attach = False
        for e in self.engs:
            self._need(e, allt)
        self.attach = sv
        self.state = {}


class Rot:
    def __init__(self, items):
        self.items = items
        self.i = 0

    def next(self):
        it = self.items[self.i]
        self.i = (self.i + 1) % len(self.items)
        return it


def build_program(S_TOK, NSEQ, DEPTH, debug=False):
    assert S_TOK % 2048 == 0
    NTOK = S_TOK * NSEQ
    NT = S_TOK // 512
    NCH = S_TOK // 128
    nc = bass.Bass("TRN2", target_bir_lowering=False)

    def din(name, shape, dt):
        return nc.dram_tensor(name, shape, dt, kind="ExternalInput").ap()

    def dscr(name, shape, dt):
        return nc.dram_tensor(name, shape, dt, kind="Internal").ap()

    x_in = din("x", [NTOK, D], F32)
    w_in = din("w_in", [DEPTH, D, NIN], F32)
    w_g = din("w_g", [DEPTH, D, 256], F32)
    w_out = din("w_out", [DEPTH, DMIX, D], F32)
    normg = din("normg", [DEPTH, 128, 8], F32)
    convw = din("convw", [DEPTH, 128, 8, 4], F32)
    convb = din("convb", [DEPTH, 128, 8], F32)
    gateb = din("gateb", [DEPTH, 128, 2], F32)
    qkg = din("qkg", [DEPTH, 128, 2], F32)
    mng = din("mng", [DEPTH, 128, 1024], F32)
    c_identb = din("c_identb", [128, 128], BF16)
    c_identf = din("c_identf", [128, 128], F32)
    c_mask = din("c_mask", [128, 768], BF16)
    c_blk = din("c_blk", [128, 128], BF16)
    c_coef = din("c_coef", [128, 4], F32)
    y_out = nc.dram_tensor("y", [NTOK, D], F32, kind="ExternalOutput").ap()

    xs = [dscr("xs0", [NTOK, D], F32), dscr("xs1", [NTOK, D], F32)]
    ws_in = dscr("ws_in", [DEPTH, 128, 8 * 6144], BF16)
    ws_g = dscr("ws_g", [DEPTH, 128, 8 * 256], BF16)
    ws_out = dscr("ws_out", [DEPTH, 128, 12 * 1024], BF16)
    s_qT = dscr("s_qT", [NSEQ, 512, S_TOK], BF16)
    s_kT = dscr("s_kT", [NSEQ, 512, S_TOK], BF16)
    s_v = dscr("s_v", [NTOK, 1024], BF16)
    s_o = dscr("s_o", [NTOK, 1024], BF16)
    s_z = dscr("s_z", [NTOK, 1024], BF16)
    s_gI = dscr("s_gI", [NSEQ, 128, S_TOK], F32)
    s_gF = dscr("s_gF", [NSEQ, 128, S_TOK], F32)
    s_aqT = dscr("s_aqT", [NSEQ, 512, S_TOK], BF16)
    s_akT = dscr("s_akT", [NSEQ, 512, S_TOK], BF16)
    s_av = dscr("s_av", [NTOK, 512], BF16)
    s_az = dscr("s_az", [NTOK, 512], BF16)
    s_hm = dscr("s_hm", [NTOK, 1024], BF16)
    s_od = dscr("s_od", [3, NTOK, 520], F32)
    s_dec = dscr("s_dec", [128, NCH], F32)
    dbg = {}
    if debug:
        dbg["colz"] = nc.dram_tensor("dbg_colz", [128, NCH * 8], F32, kind="ExternalOutput").ap()
        dbg["decb"] = nc.dram_tensor("dbg_decb", [128, 4 * NCH], F32, kind="ExternalOutput").ap()

    with ExitStack() as top:
        S = Sched(nc, top)

        uniq = [0]

        def sbt(es, name, shape, dt):
            uniq[0] += 1
            return es.enter_context(nc.sbuf_tensor("%s_u%d" % (name, uniq[0]), shape, dt))

        def pst(es, name, shape, dt):
            uniq[0] += 1
            return es.enter_context(nc.psum_tensor("%s_u%d" % (name, uniq[0]), shape, dt))

        identb = sbt(top, "identb", [128, 128], BF16)
        identf = sbt(top, "identf", [128, 128], F32)
        maskt = sbt(top, "maskt", [128, 768], BF16)
        blk = sbt(top, "blk", [128, 128], BF16)
        coef = sbt(top, "coef", [128, 4], F32)
        colz = sbt(top, "colz", [128, NCH, 4, 2], F32)
        decb = sbt(top, "decb", [128, 4, NCH], F32)
        S.dma('sp', identb[:], c_identb[:, :], writes=["identb"])
        S.dma('sp', identf[:], c_identf[:, :], writes=["identf"])
        S.dma('sp', maskt[:], c_mask[:, :], writes=["maskt"])
        S.dma('sp', blk[:], c_blk[:, :], writes=["blk"])
        S.dma('sp', coef[:], c_coef[:, :], writes=["coef"])
        e0c, e1c, cstc, epsc = coef[:, 0:1], coef[:, 1:2], coef[:, 2:3], coef[:, 3:4]

        def weight_pieces(l):
            out = []
            for kc in range(8):
                rows = slice(kc * 128, (kc + 1) * 128)
                out.append((w_g[l, rows, :], ws_g[l, :, kc * 256:(kc + 1) * 256], 256, kc))
                out.append((w_in[l, rows, 4104:6152], ws_in[l, :, kc * 6144 + 4096:kc * 6144 + 6144], 2048, kc))
                out.append((w_in[l, rows, 0:2048], ws_in[l, :, kc * 6144:kc * 6144 + 2048], 2048, kc))
                out.append((w_in[l, rows, 2048:4096], ws_in[l, :, kc * 6144 + 2048:kc * 6144 + 4096], 2048, kc))
            for mc in range(12):
                out.append((w_out[l, mc * 128:(mc + 1) * 128, :], ws_out[l, :, mc * 1024:(mc + 1) * 1024], 1024, None))
            return out

        class Caster:
            def __init__(self, es, l, tag):
                self.l = l
                self.ng = sbt(es, tag + "_ng", [128, 8], F32)
                self.ngk = tag + "_ng"
                S.dma('sp', self.ng[:], normg[l], writes=[self.ngk])
                self.stg = Rot([(sbt(es, "%s_stg%d" % (tag, i), [128, 2048], F32), "%s_stg%d" % (tag, i)) for i in range(2)])
                self.obf = Rot([(sbt(es, "%s_obf%d" % (tag, i), [128, 2048], BF16), "%s_obf%d" % (tag, i)) for i in range(2)])
                self.k = 0
                self.pstore = None

            def flush(self):
                if self.pstore is not None:
                    (dst, ob, obk, n) = self.pstore
                    S.dma('sp', dst, ob[:, 0:n], reads=[obk])
                    self.pstore = None

            def run(self, piece):
                (src, dst, n, kc) = piece
                st, stk = self.stg.next()
                ob, obk = self.obf.next()
                S.dma('sp', st[:, 0:n], src, writes=[stk])
                self.flush()
                eng = 'dve' if self.k % 2 == 0 else 'act'
                self.k += 1
                if kc is None:
                    if eng == 'act':
                        S.op('act', lambda e: e.copy(out=ob[:, 0:n], in_=st[:, 0:n]), reads=[stk], writes=[obk])
                    else:
                        S.op('dve', lambda e: e.tensor_copy(out=ob[:, 0:n], in_=st[:, 0:n]), reads=[stk], writes=[obk])
                else:
                    gcol = self.ng[:, kc:kc + 1]
                    if eng == 'act':
                        S.op('act', lambda e: e.activation(out=ob[:, 0:n], in_=st[:, 0:n], func=AF.Copy, scale=gcol), reads=[stk, self.ngk], writes=[obk])
                    else:
                        S.op('dve', lambda e: e.tensor_scalar(out=ob[:, 0:n], in0=st[:, 0:n], scalar1=gcol, scalar2=None, op0=ALU.mult),
                             reads=[stk, self.ngk], writes=[obk])
                self.pstore = (dst, ob, obk, n)

        def prologue():
            with ExitStack() as es:
                cst = Caster(es, 0, "pl")
                for p in weight_pieces(0):
                    cst.run(p)
                cst.flush()
                S.barrier()

        def phase_A(l, s, xsrc):
            t0 = s * S_TOK
            with ExitStack() as es:
                Wb = sbt(es, "A_Wb", [128, 8, 6144], BF16)
                Wg = sbt(es, "A_Wg", [128, 8, 256], BF16)
                cw = sbt(es, "A_cw", [128, 8, 4], F32)
                cb = sbt(es, "A_cb", [128, 8], F32)
                gb = sbt(es, "A_gb", [128, 2], F32)
                qg = sbt(es, "A_qg", [128, 2], F32)
                mg = sbt(es, "A_mg", [128, 1024], F32)
                xt = Rot([(sbt(es, "A_xt%d" % i, [128, 1024], F32), "A_xt%d" % i) for i in range(4)])
                hT = Rot([(sbt(es, "A_hT%d" % i, [128, 8, 512], BF16), "A_hT%d" % i) for i in range(2)])
                sqj = sbt(es, "A_sqj", [128, 1024], BF16)
                ssq = Rot([(sbt(es, "A_ssq%d" % i, [128, 2], F32), "A_ssq%d" % i) for i in range(4)])
                xn = Rot([(sbt(es, "A_xn%d" % i, [128, 1024], BF16), "A_xn%d" % i) for i in range(4)])
                cbuf = sbt(es, "A_cbuf", [128, 8, 515], F32)
                acc = Rot([(sbt(es, "A_acc%d" % i, [128, 512], F32), "A_acc%d" % i) for i in range(2)])
                fmo = Rot([(sbt(es, "A_fmo%d" % i, [128, 512], BF16), "A_fmo%d" % i) for i in range(4)])
                tmo = Rot([(sbt(es, "A_tmo%d" % i, [128, 1024], BF16), "A_tmo%d" % i) for i in range(4)])
                tmz = Rot([(sbt(es, "A_tmz%d" % i, [128, 1024], BF16), "A_tmz%d" % i) for i in range(2)])
                gto = Rot([(sbt(es, "A_gto%d" % i, [128, 512], F32), "A_gto%d" % i) for i in range(2)])
                rsb = Rot([(sbt(es, "A_rs%d" % i, [128, 512], F32), "A_rs%d" % i) for i in range(2)])
                sqb = Rot([(sbt(es, "A_sqb%d" % i, [128, 512], BF16), "A_sqb%d" % i) for i in range(2)])
                pmain = Rot([(pst(es, "A_pm%d" % i, [128, 512], F32), "A_pm%d" % i) for i in range(5)])
                pss = Rot([(pst(es, "A_pss%d" % i, [128, 512], F32), "A_pss%d" % i) for i in range(2)])
                ptr = pst(es, "A_ptr", [128, 1024], BF16)
                S.op('pool', lambda e: e.memset(cbuf[:, :, 0:3], 0.0), writes=[("cbuf", c) for c in range(8)])

                def front_load(i):
                    lds = []
                    for j in range(4):
                        xtile, xk = xt.next()
                        S.dma('sp', xtile[:], xsrc[t0 + i * 512 + j * 128:t0 + i * 512 + (j + 1) * 128, :], writes=[xk])
                        lds.append((xtile, xk))
                    return lds

                def front_a(lds):
                    xns = []
                    for j in range(4):
                        xtile, xk = lds[j]
                        sq, sqk = ssq.next()
                        xnt, xnk = xn.next()
                        S.op('act', lambda e: e.activation(out=sqj[:], in_=xtile[:], func=AF.Square, accum_out=sq[:, 0:1]),
                             reads=[xk], writes=["sqj", sqk])
                        S.op('act', lambda e: e.activation(out=sq[:, 1:2], in_=sq[:, 0:1], func=AF.Ln, bias=epsc, scale=1.0 / D),
                             reads=[sqk, "coef"], writes=[sqk])
                        S.op('act', lambda e: e.activation(out=sq[:, 1:2], in_=sq[:, 1:2], func=AF.Exp, scale=-0.5), reads=[sqk], writes=[sqk])
                        S.op('dve', lambda e: e.tensor_scalar(out=xnt[:], in0=xtile[:], scalar1=sq[:, 1:2], scalar2=None, op0=ALU.mult),
                             reads=[xk, sqk], writes=[xnk])
                        xns.append((xnt, xnk))
                    return xns

                def front_b(xns):
                    hTt, hk = hT.next()
                    for j in range(4):
                        xnt, xnk = xns[j]
                        for kc in range(8):
                            S.op('pe', lambda e: e.transpose(ptr[:, kc * 128:(kc + 1) * 128], xnt[:, kc * 128:(kc + 1) * 128], identb[:]),
                                 reads=[xnk, "identb"], writes=["ptr"], signal=(kc == 7))
                        if j % 2:
                            S.op('act', lambda e: e.copy(out=hTt[:, :, j * 128:(j + 1) * 128], in_=ptr[:].rearrange("p (k t) -> p k t", k=8)),
                                 reads=["ptr"], writes=[hk])
                        else:
                            S.op('dve', lambda e: e.tensor_copy(out=hTt[:, :, j * 128:(j + 1) * 128], in_=ptr[:].rearrange("p (k t) -> p k t", k=8)),
                                 reads=["ptr"], writes=[hk])
                    return hTt, hk

                def mm_fm(hTt, hk, wtile, wkeys, col0):
                    if wkeys is None:
                        wkeys = [("Wb", col0 // 512)]
                    ps, pk = pmain.next()
                    for kc in range(8):
                        S.op('pe', lambda e: e.matmul(ps[:], lhsT=wtile[:, kc, col0:col0 + 128], rhs=hTt[:, kc, :], start=(kc == 0), stop=(kc == 7)),
                             reads=[hk] + wkeys, writes=[pk], signal=(kc == 7))
                    return ps, pk

                def mm_tm(hTt, hk, j, col0):
                    ps, pk = pmain.next()
                    for kc in range(8):
                        S.op('pe', lambda e: e.matmul(ps[:], lhsT=hTt[:, kc, j * 128:(j + 1) * 128], rhs=Wb[:, kc, col0:col0 + 512], start=(kc == 0), stop=(kc == 7)),
                             reads=[hk, ("Wb", col0 // 512)], writes=[pk], signal=(kc == 7))
                    return ps, pk

                def tile_groups(i, hTt, hk, mid):
                    tok0 = i * 512
                    deferred = []

                    def run_deferred():
                        while deferred:
                            deferred.pop(0)()

                    def conv_group(c):
                        ps, pk = mm_fm(hTt, hk, Wb, None, c * 128)
                        run_deferred()
                        ck = ("cbuf", c)
                        a, ak = acc.next()
                        S.op('act', lambda e: e.copy(out=cbuf[:, c, 3:515], in_=ps[:]), reads=[pk], writes=[ck])
                        S.op('dve', lambda e: e.tensor_scalar(out=a[:], in0=cbuf[:, c, 0:512], scalar1=cw[:, c, 0:1], scalar2=None, op0=ALU.mult),
                             reads=[ck, "cw"], writes=[ak])
                        for jt in range(1, 4):
                            S.op('dve', lambda e: e.scalar_tensor_tensor(out=a[:], in0=cbuf[:, c, jt:jt + 512], scalar=cw[:, c, jt:jt + 1], in1=a[:],
                                                                         op0=ALU.mult, op1=ALU.add),
                                 reads=[ck, "cw", ak], writes=[ak])
                        fo, fk = fmo.next()
                        S.op('act', lambda e: e.activation(out=fo[:], in_=a[:], func=AF.Silu, bias=cb[:, c:c + 1]), reads=[ak, "cb"], writes=[fk])
                        S.op('pool', lambda e: e.tensor_copy(out=cbuf[:, c, 0:3], in_=cbuf[:, c, 512:515]), reads=[ck], writes=[ck])
                        dst = (s_qT if c < 4 else s_kT)[s, (c % 4) * 128:(c % 4 + 1) * 128, tok0:tok0 + 512]
                        S.dma('sp', dst, fo[:], reads=[fk])

                    def norm_group(c):
                        ps, pk = mm_fm(hTt, hk, Wb, None, 4096 + c * 128)
                        run_deferred()
                        sqt, sqk = sqb.next()
                        S.op('act', lambda e: e.activation(out=sqt[:], in_=ps[:], func=AF.Square), reads=[pk], writes=[sqk])

                        def second():
                            p2, p2k = pss.next()
                            S.op('pe', lambda e: e.matmul(p2[:], lhsT=blk[:], rhs=sqt[:], start=True, stop=True), reads=["blk", sqk], writes=[p2k])
                            rs, rk = rsb.next()
                            S.op('act', lambda e: e.activation(out=rs[:], in_=p2[:], func=AF.Ln, bias=epsc, scale=1.0 / 64), reads=[p2k, "coef"], writes=[rk])
                            S.op('act', lambda e: e.activation(out=rs[:], in_=rs[:], func=AF.Exp, scale=-0.5), reads=[rk], writes=[rk])
                            fo, fk = fmo.next()
                            gcol = qg[:, 0:1] if c < 4 else qg[:, 1:2]
                            S.op('dve', lambda e: e.scalar_tensor_tensor(out=fo[:], in0=ps[:], scalar=gcol, in1=rs[:], op0=ALU.mult, op1=ALU.mult),
                                 reads=[pk, "qg", rk], writes=[fk])
                            dst = (s_aqT if c < 4 else s_akT)[s, (c % 4) * 128:(c % 4 + 1) * 128, tok0:tok0 + 512]
                            S.dma('sp', dst, fo[:], reads=[fk])
                        deferred.append(second)

                    def gate_group(which):
                        ps, pk = mm_fm(hTt, hk, Wg, ["Wg"], which * 128)
                        run_deferred()
                        go, gk = gto.next()
                        S.op('act', lambda e: e.activation(out=go[:], in_=ps[:], func=AF.Identity, bias=gb[:, which:which + 1]), reads=[pk, "gb"], writes=[gk])
                        dst = (s_gI if which == 0 else s_gF)[s, :, tok0:tok0 + 512]
                        S.dma('sp', dst, go[:], reads=[gk])

                    def rows_of(j):
                        return slice(t0 + tok0 + j * 128, t0 + tok0 + (j + 1) * 128)

                    def tm_v(j):
                        to, tk = tmo.next()
                        for b in range(2):
                            ps, pk = mm_tm(hTt, hk, j, 1024 + b * 512)
                            run_deferred()
                            if b == 0:
                                S.op('dve', lambda e: e.tensor_copy(out=to[:, 0:512], in_=ps[:]), reads=[pk], writes=[tk])
                            else:
                                S.op('act', lambda e: e.copy(out=to[:, 512:1024], in_=ps[:]), reads=[pk], writes=[tk])
                        S.dma('sp', s_v[rows_of(j), :], to[:], reads=[tk])

                    def tm_o(j):
                        to, tk = tmo.next()
                        for b in range(2):
                            ps, pk = mm_tm(hTt, hk, j, 2048 + b * 512)
                            run_deferred()
                            S.op('act', lambda e: e.activation(out=to[:, b * 512:(b + 1) * 512], in_=ps[:], func=AF.Sigmoid), reads=[pk], writes=[tk])
                        S.dma('sp', s_o[rows_of(j), :], to[:], reads=[tk])

                    def tm_z(j):
                        to, tk = tmo.next()
                        tz, tzk = tmz.next()
                        for b in range(2):
                            ps, pk = mm_tm(hTt, hk, j, 3072 + b * 512)
                            run_deferred()
                            S.op('act', lambda e: e.activation(out=to[:, b * 512:(b + 1) * 512], in_=ps[:], func=AF.Silu), reads=[pk], writes=[tk])
                        S.op('pool', lambda e: e.tensor_tensor(out=tz[:], in0=to[:], in1=mg[:], op=ALU.mult), reads=[tk, "mg"], writes=[tzk])
                        S.dma('sp', s_z[rows_of(j), :], tz[:], reads=[tzk])

                    def tm_a(j):
                        to, tk = tmo.next()
                        ps, pk = mm_tm(hTt, hk, j, 5120)
                        run_deferred()
                        S.op('dve', lambda e: e.tensor_copy(out=to[:, 0:512], in_=ps[:]), reads=[pk], writes=[tk])
                        ps, pk = mm_tm(hTt, hk, j, 5632)
                        run_deferred()
                        S.op('act', lambda e: e.activation(out=to[:, 512:1024], in_=ps[:], func=AF.Silu), reads=[pk], writes=[tk])
                        S.dma('sp', s_av[rows_of(j), :], to[:, 0:512], reads=[tk])
                        S.dma('sp', s_az[rows_of(j), :], to[:, 512:1024], reads=[tk])

                    gate_group(0)
                    gate_group(1)
                    for c in range(8):
                        norm_group(c)
                    for j in range(4):
                        tm_o(j)
                    if mid is not None:
                        mid()
                    for j in range(4):
                        conv_group(2 * j)
                        tm_z(j)
                        tm_v(j)
                        conv_group(2 * j + 1)
                        tm_a(j)
                    run_deferred()

                ld = {0: front_load(0)}
                S.dma('sp', cw[:], convw[l], writes=["cw"])
                S.dma('sp', cb[:], convb[l], writes=["cb"])
                S.dma('sp', gb[:], gateb[l], writes=["gb"])
                S.dma('sp', qg[:], qkg[l], writes=["qg"])
                S.dma('sp', mg[:], mng[l], writes=["mg"])
                xns0 = front_a(ld[0])
                S.dma('sp', Wg[:].rearrange("p k n -> p (k n)"), ws_g[l], writes=["Wg"])
                wsv = ws_in[l].rearrange("p (k n) -> p k n", k=8)
                for wcb in (8, 9, 4, 5, 0, 1, 6, 7, 2, 3, 10, 11):
                    S.dma('sp', Wb[:, :, wcb * 512:(wcb + 1) * 512], wsv[:, :, wcb * 512:(wcb + 1) * 512], writes=[("Wb", wcb)])
                if NT > 1:
                    ld[1] = front_load(1)
                cur = front_b(xns0)
                for i in range(NT):
                    nxt = {}
                    xns = front_a(ld[i + 1]) if i + 1 < NT else None

                    def mid():
                        if xns is not None:
                            nxt["v"] = front_b(xns)
                        if i + 2 < NT:
                            ld[i + 2] = front_load(i + 2)
                    tile_groups(i, cur[0], cur[1], mid)
                    cur = nxt.get("v")
                S.barrier()

        def phase_B(l, s):
            with ExitStack() as es:
                I = sbt(es, "B_I", [128, S_TOK], F32)
                Fr = sbt(es, "B_F", [128, S_TOK], F32)
                nB = sbt(es, "B_nB", [128, S_TOK], F32)
                M = sbt(es, "B_M", [128, S_TOK], F32)
                ones1 = sbt(es, "B_ones", [128, 1], F32)
                dl = sbt(es, "B_dl", [128, NCH], F32)
                ptb = Rot([(pst(es, "B_pt%d" % i, [128, 512], F32), "B_pt%d" % i) for i in range(2)])
                S.op('pool', lambda e: e.memset(ones1[:], 1.0), writes=["ones1"])
                S.dma('sp', I[:], s_gI[s], writes=["I"])
                S.dma('sp', Fr[:], s_gF[s], writes=["F"])
                S.op('act', lambda e: e.activation(out=Fr[:], in_=Fr[:], func=AF.Exp, scale=-1.0), reads=["F"], writes=["F"])
                S.op('act', lambda e: e.activation(out=Fr[:], in_=Fr[:], func=AF.Ln, bias=1.0), reads=["F"], writes=["F"])
                onesb = ones1[:, 0:1].broadcast_to([128, S_TOK])
                S.op('dve', lambda e: e.tensor_tensor_scan(out=nB[:], data0=onesb, data1=Fr[:], initial=0.0, op0=ALU.mult, op1=ALU.add),
                     reads=["F", "ones1"], writes=["nB"])
                S.op('dve', lambda e: e.tensor_tensor(out=I[:], in0=I[:], in1=nB[:], op=ALU.add), reads=["I", "nB"], writes=["I"])
                S.op('dve', lambda e: e.tensor_tensor_scan(out=M[:], data0=onesb, data1=I[:], initial=0.0, op0=ALU.mult, op1=ALU.max),
                     reads=["I", "ones1"], writes=["M"])
                S.op('dve', lambda e: e.tensor_scalar(out=Fr[:], in0=I[:], scalar1=e0c, scalar2=cstc, op0=ALU.mult, op1=ALU.add),
                     reads=["I", "coef"], writes=["F"])
                S.op('dve', lambda e: e.scalar_tensor_tensor(out=Fr[:], in0=nB[:], scalar=e1c, in1=Fr[:], op0=ALU.mult, op1=ALU.add),
                     reads=["nB", "coef", "F"], writes=["F"])
                Mv = M[:].rearrange("p (c j) -> p c j", j=128)
                Mlast_b = Mv[:, :, 127:128].broadcast_to([128, NCH, 128])
                S.op('dve', lambda e: e.tensor_tensor(out=nB[:].rearrange("p (c j) -> p c j", j=128), in0=Fr[:].rearrange("p (c j) -> p c j", j=128),
                                                      in1=Mlast_b, op=ALU.subtract), reads=["F", "M"], writes=["nB"])
                S.op('act', lambda e: e.activation(out=I[:], in_=nB[:], func=AF.Exp), reads=["nB"], writes=["I"])
                for c4 in range(NCH // 4):
                    pt, ptk = ptb.next()
                    for k in range(4):
                        c = c4 * 4 + k
                        S.op('pe', lambda e: e.transpose(pt[:, k * 128:(k + 1) * 128], I[:, c * 128:(c + 1) * 128], identf[:]),
                             reads=["I", "identf"], writes=[ptk], signal=(k == 3))
                    S.op('dve', lambda e: e.tensor_copy(out=colz[:, c4 * 4:(c4 + 1) * 4, :, :],
                                                        in_=pt[:].rearrange("p (k h r) -> p k h r", k=4, h=4)[:, :, :, 0:2]),
                         reads=[ptk], writes=["colz"])
                Ml = M[:, 127:S_TOK:128]
                S.op('dve', lambda e: e.tensor_scalar(out=dl[:, 0:1], in0=M[:, 127:128], scalar1=-1.0, scalar2=None, op0=ALU.mult), reads=["M"], writes=["dl"])
                if NCH > 1:
                    S.op('dve', lambda e: e.tensor_tensor(out=dl[:, 1:NCH], in0=M[:, 127:S_TOK - 128:128], in1=M[:, 255:S_TOK:128], op=ALU.subtract),
                         reads=["M"], writes=["dl"])
                S.op('act', lambda e: e.activation(out=dl[:], in_=dl[:], func=AF.Exp), reads=["dl"], writes=["dl"])
                S.dma('sp', s_dec[:, :], dl[:], reads=["dl"], writes=["s_dec"])
                for h in range(4):
                    S.dma('sp', decb[:, h, :], s_dec[32 * h:32 * h + 1, :].broadcast_to([128, NCH]), reads=["s_dec"], writes=["decb"])
                if debug:
                    S.dma('sp', dbg["colz"][:, :], colz[:].rearrange("p c h r -> p (c h r)"), reads=["colz"])
                    S.dma('sp', dbg["decb"][:, :], decb[:].rearrange("p h c -> p (h c)"), reads=["decb"])
                S.barrier()

        def phase_C(l, s):
            t0 = s * S_TOK
            NG = NCH // 4
            with ExitStack() as es:
                qTg = Rot([(sbt(es, "C_q%d" % i, [128, 4, 512], BF16), "C_q%d" % i) for i in range(2)])
                kTg = Rot([(sbt(es, "C_k%d" % i, [128, 4, 512], BF16), "C_k%d" % i) for i in range(2)])
                vg = Rot([(sbt(es, "C_v%d" % i, [128, 4, 4, 258], BF16), "C_v%d" % i) for i in range(2)])
                og = Rot([(sbt(es, "C_o%d" % i, [128, 4, 1024], BF16), "C_o%d" % i) for i in range(2)])
                zg = Rot([(sbt(es, "C_z%d" % i, [128, 4, 1024], BF16), "C_z%d" % i) for i in range(2)])
                hmg = Rot([(sbt(es, "C_hm%d" % i, [128, 4, 1024], BF16), "C_hm%d" % i) for i in range(2)])
                Cm = sbt(es, "C_Cm", [128, 4, 257], F32)
                Cb = sbt(es, "C_Cb", [128, 4, 258], BF16)
                STt = Rot([(sbt(es, "C_ST%d" % i, [128, 4, 128], BF16), "C_ST%d" % i) for i in range(2)])
                kwt = Rot([(sbt(es, "C_kw%d" % i, [128, 4, 128], BF16), "C_kw%d" % i) for i in range(2)])
                t1t = Rot([(sbt(es, "C_t1%d" % i, [128, 4, 256], F32), "C_t1%d" % i) for i in range(2)])
                sqj = sbt(es, "C_sqj", [128, 256], BF16)
                sm = Rot([(sbt(es, "C_sm%d" % i, [128, 16], F32), "C_sm%d" % i) for i in range(3)])
                ps_s = pst(es, "C_pss", [128, 512], F32)
                ps_k = pst(es, "C_psk", [128, 1024], BF16)
                ps_o = pst(es, "C_pso", [128, 4, 512], F32)
                ps_c = Rot([(pst(es, "C_psc%d" % i, [128, 512], F32), "C_psc%d" % i) for i in range(2)])
                S.op('pool', lambda e: e.memset(Cm[:], 0.0), writes=[("Cm", h) for h in range(4)])
                for (vt, vk) in vg.items:
                    S.op('pool', lambda e: e.memset(vt[:], 1.0), writes=[vk])

                def load_group(g):
                    q, qk_ = qTg.next()
                    k, kk_ = kTg.next()
                    v, vk = vg.next()
                    o, ok = og.next()
                    z, zk = zg.next()
                    tk0 = g * 512
                    S.dma('sp', q[:], s_qT[s, :, tk0:tk0 + 512].rearrange("(h p) t -> p h t", p=128), writes=[qk_])
                    S.dma('sp', k[:], s_kT[s, :, tk0:tk0 + 512].rearrange("(h p) t -> p h t", p=128), writes=[kk_])
                    rows = slice(t0 + tk0, t0 + tk0 + 512)
                    for cc in range(4):
                        r4 = slice(t0 + tk0 + cc * 128, t0 + tk0 + (cc + 1) * 128)
                        S.dma('sp', v[:, cc, :, 0:256], s_v[r4, :].rearrange("p (h e) -> p h e", h=4), writes=[vk])
                    S.dma('sp', o[:], s_o[rows, :].rearrange("(c p) n -> p c n", p=128), writes=[ok])
                    S.dma('sp', z[:], s_z[rows, :].rearrange("(c p) n -> p c n", p=128), writes=[zk])
                    return (q, qk_, k, kk_, v, vk, o, ok, z, zk)

                pend = []

                def stage5():
                    (t1, t1k, smt, smk, z, zk, cc, hm, hmk, g, last) = pend.pop(0)
                    for h in range(4):
                        S.op('dve', lambda e: e.scalar_tensor_tensor(out=hm[:, cc, h * 256:(h + 1) * 256], in0=t1[:, h, :], scalar=smt[:, 12 + h:13 + h],
                                                                     in1=z[:, cc, h * 256:(h + 1) * 256], op0=ALU.mult, op1=ALU.mult),
                             reads=[(t1k, h), smk, zk], writes=[hmk])
                    if last:
                        rows = slice(t0 + g * 512, t0 + (g + 1) * 512)
                        S.dma('sp', s_hm[rows, :].rearrange("(c p) n -> p c n", p=128), hm[:], reads=[hmk])

                nxt = load_group(0)
                for g in range(NG):
                    (q, qk_, k, kk_, v, vk, o, ok, z, zk) = nxt
                    hm, hmk = hmg.next()
                    for cc in range(4):
                        c = g * 4 + cc
                        ts = slice(cc * 128, (cc + 1) * 128)
                        st, stk = STt.next()
                        kw, kwk = kwt.next()
                        t1, t1k = t1t.next()
                        smt, smk = sm.next()
                        for h in range(4):
                            S.op('pe', lambda e: e.matmul(ps_s[:, h * 128:(h + 1) * 128], lhsT=k[:, h, ts], rhs=q[:, h, ts], start=True, stop=True),
                                 reads=[kk_, qk_], writes=["C_pss"], signal=(h == 3))
                        for h in range(4):
                            S.op('pe', lambda e: e.transpose(ps_k[:, h * 128:(h + 1) * 128], k[:, h, ts], identb[:]), reads=[kk_, "identb"], writes=["C_psk"],
                                 signal=(h == 3))
                        for h in range(4):
                            S.op('dve', lambda e: e.scalar_tensor_tensor(out=st[:, h, :], in0=ps_s[:, h * 128:(h + 1) * 128], scalar=colz[:, c, h, 0:1],
                                                                         in1=maskt[:, 0:128], op0=ALU.mult, op1=ALU.mult),
                                 reads=["C_pss", "colz", "maskt"], writes=[(stk, h)])
                        for h in range(4):
                            S.op('act', lambda e: e.activation(out=kw[:, h, :], in_=ps_k[:, h * 128:(h + 1) * 128], func=AF.Copy, scale=colz[:, c, h, 0:1]),
                                 reads=["C_psk", "colz"], writes=[(kwk, h)])
                        for h in range(4):
                            S.op('act', lambda e: e.activation(out=Cb[:, h, 0:257], in_=Cm[:, h, :], func=AF.Copy, scale=decb[:, h, c:c + 1]),
                                 reads=[("Cm", h), "decb"], writes=[("Cb", h)])
                        if pend:
                            stage5()
                        if cc == 0 and g + 1 < NG:
                            nxt = load_group(g + 1)
                        pcs = []
                        for h in range(4):
                            vext = v[:, cc, h, 0:257]
                            S.op('pe', lambda e: e.matmul(ps_o[:, h, 0:257], lhsT=st[:, h, :], rhs=vext, start=True, stop=False), reads=[(stk, h), vk],
                                 writes=[("C_pso", h)], signal=False)
                            S.op('pe', lambda e: e.matmul(ps_o[:, h, 0:257], lhsT=q[:, h, ts], rhs=Cb[:, h, 0:257], start=False, stop=True),
                                 reads=[qk_, ("Cb", h)], writes=[("C_pso", h)])
                            psc, psck = ps_c.next()
                            S.op('pe', lambda e: e.matmul(psc[:, 0:257], lhsT=kw[:, h, :], rhs=vext, start=True, stop=True), reads=[(kwk, h), vk], writes=[psck])
                            S.op('dve', lambda e: e.scalar_tensor_tensor(out=Cm[:, h, :], in0=Cm[:, h, :], scalar=decb[:, h, c:c + 1], in1=psc[:, 0:257],
                                                                         op0=ALU.mult, op1=ALU.add),
                                 reads=[("Cm", h), "decb", psck], writes=[("Cm", h)])
                        pso_keys = [("C_pso", h) for h in range(4)]
                        denv = ps_o[:, :, 256:257]
                        S.op('dve', lambda e: e.tensor_scalar(out=smt[:, 0:4].rearrange("p (h o) -> p h o", o=1), in0=denv, scalar1=-1.0, scalar2=None, op0=ALU.mult),
                             reads=pso_keys, writes=[smk])
                        S.op('dve', lambda e: e.tensor_tensor(out=smt[:, 0:4], in0=smt[:, 0:4], in1=colz[:, c, :, 1], op=ALU.max), reads=[smk, "colz"], writes=[smk])
                        S.op('dve', lambda e: e.tensor_tensor(out=smt[:, 0:4].rearrange("p (h o) -> p h o", o=1), in0=denv,
                                                              in1=smt[:, 0:4].rearrange("p (h o) -> p h o", o=1), op=ALU.max), reads=pso_keys + [smk], writes=[smk])
                        S.op('dve', lambda e: e.reciprocal(out=smt[:, 4:8], in_=smt[:, 0:4]), reads=[smk], writes=[smk])
                        for h in range(4):
                            S.op('dve', lambda e: e.scalar_tensor_tensor(out=t1[:, h, :], in0=ps_o[:, h, 0:256], scalar=smt[:, 4 + h:5 + h],
                                                                         in1=o[:, cc, h * 256:(h + 1) * 256], op0=ALU.mult, op1=ALU.mult),
                                 reads=[("C_pso", h), smk, ok], writes=[(t1k, h)])
                        for h in range(4):
                            S.op('act', lambda e: e.activation(out=sqj[:], in_=t1[:, h, :], func=AF.Square, accum_out=smt[:, 8 + h:9 + h]),
                                 reads=[(t1k, h)], writes=["C_sqj", smk])
                        S.op('act', lambda e: e.activation(out=smt[:, 12:16], in_=smt[:, 8:12], func=AF.Ln, bias=epsc, scale=1.0 / 256), reads=[smk, "coef"], writes=[smk])
                        S.op('act', lambda e: e.activation(out=smt[:, 12:16], in_=smt[:, 12:16], func=AF.Exp, scale=-0.5), reads=[smk], writes=[smk])
                        pend.append((t1, t1k, smt, smk, z, zk, cc, hm, hmk, g, cc == 3))
                while pend:
                    stage5()
                S.barrier()

        def phase_D(l, s):
            t0 = s * S_TOK
            with ExitStack() as es:
                QT = [sbt(es, "D_Q%d" % j, [128, 2, S_TOK], BF16) for j in range(4)]
                KT = [sbt(es, "D_K%d" % j, [128, S_TOK], BF16) for j in range(4)]
                NB = S_TOK // 128
                Vd = Rot([(sbt(es, "D_V%d" % i, [128, NB, 8, 66], BF16), "D_V%d" % i) for i in range(2)])
                Vstg = Rot([(sbt(es, "D_Vstg%d" % i, [128, 8, 512], BF16), "D_Vstg%d" % i) for i in range(2)])
                pex = Rot([(sbt(es, "D_pe%d" % i, [128, 512], BF16), "D_pe%d" % i) for i in range(4)])
                pmk = Rot([(sbt(es, "D_pm%d" % i, [128, 512], BF16), "D_pm%d" % i) for i in range(4)])
                ostg = Rot([(sbt(es, "D_os%d" % i, [128, 8, 65], F32), "D_os%d" % i) for i in range(3)])
                ps_s = Rot([(pst(es, "D_pss%d" % i, [128, 512], F32), "D_pss%d" % i) for i in range(4)])
                ps_o = Rot([(pst(es, "D_pso%d" % i, [128, 512], F32), "D_pso%d" % i) for i in range(4)])
                mask4 = maskt[:, 256:768]
                for j in range(4):
                    S.op('dve', lambda e: e.memset(QT[j][64:128, 0, :], 0.0), writes=[("QT", j, "z0")])
                    S.op('pool', lambda e: e.memset(QT[j][0:64, 1, :], 0.0), writes=[("QT", j, "z1")])
                    S.dma('sp', QT[j][0:64, 0, :], s_aqT[s, j * 128:j * 128 + 64, :], writes=[("QT", j, "a")])
                    S.dma('sp', QT[j][64:128, 1, :], s_aqT[s, j * 128 + 64:(j + 1) * 128, :], writes=[("QT", j, "b")])
                    S.dma('sp', KT[j][:], s_akT[s, j * 128:(j + 1) * 128, :], writes=[("KT", j)])
                for (vt, vk) in Vd.items:
                    S.op('pool', lambda e: e.memset(vt[:, :, :, 64:66], 1.0), writes=[(vk, "ones")])
                av_seq = s_av[t0:t0 + S_TOK, :]
                vcnt = [0]

                def load_V(d):
                    nbd_ = S_TOK // d // 128
                    V_, Vk_ = Vd.next()
                    srcv = av_seq.rearrange("(nb i dd) n -> dd i nb n", dd=d, i=128)
                    for b0 in range(0, NB, 8):
                        vs, vsk = Vstg.next()
                        if nbd_ >= 8:
                            r_, n0 = b0 // nbd_, b0 % nbd_
                            S.dma('sp', vs[:], srcv[r_][:, n0:n0 + 8, :], writes=[vsk])
                        else:
                            for r_ in range(b0 // nbd_, (b0 + 8) // nbd_):
                                o_ = r_ * nbd_ - b0
                                S.dma('sp', vs[:, o_:o_ + nbd_, :], srcv[r_], writes=[vsk])
                        eng = 'dve' if vcnt[0] % 2 == 0 else 'act'
                        vcnt[0] += 1
                        if eng == 'dve':
                            S.op('dve', lambda e: e.tensor_copy(out=V_[:, b0:b0 + 8, :, 0:64], in_=vs[:].rearrange("p b (h e) -> p b h e", h=8)),
                                 reads=[vsk], writes=[(Vk_, b0)])
                        else:
                            S.op('act', lambda e: e.copy(out=V_[:, b0:b0 + 8, :, 0:64], in_=vs[:].rearrange("p b (h e) -> p b h e", h=8)),
                                 reads=[vsk], writes=[(Vk_, b0)])
                    return V_, Vk_

                nextV = load_V(PATTERNS[0])
                for pi, d in enumerate(PATTERNS):
                    nbd = S_TOK // d // 128
                    V, Vk = nextV
                    od_seq = s_od[pi, t0:t0 + S_TOK, :].rearrange("(nb i dd) n -> dd nb i n", dd=d, i=128)
                    units = []
                    for r in range(d):
                        for nb in range(nbd):
                            for p in range(4):
                                units.append((r, nb, p))
                    pending = []
                    state = {}

                    def tok(r, nb):
                        st_ = r + d * 128 * nb
                        return slice(st_, st_ + d * 127 + 1, d)

                    def emit_qk(u):
                        r, nb, p = u
                        qkeys = [("QT", p, x) for x in ("z0", "z1", "a", "b")] + [("KT", p)]
                        pss, pssk = ps_s.next()
                        W = 512 if nb > 0 else 256
                        S.op('pe', lambda e: e.matmul(pss[:, 0:256].rearrange("p (a i) -> p a i", a=2), lhsT=KT[p][:, tok(r, nb)], rhs=QT[p][:, :, tok(r, nb)],
                                                      start=True, stop=True), reads=qkeys, writes=[pssk], signal=(nb == 0))
                        if nb > 0:
                            S.op('pe', lambda e: e.matmul(pss[:, 256:512].rearrange("p (a i) -> p a i", a=2), lhsT=KT[p][:, tok(r, nb - 1)], rhs=QT[p][:, :, tok(r, nb)],
                                                          start=True, stop=True), reads=qkeys, writes=[pssk])
                        px, pxk = pex.next()
                        S.op('act', lambda e: e.activation(out=px[:, 0:W], in_=pss[:, 0:W], func=AF.Exp, scale=0.125), reads=[pssk], writes=[pxk])
                        pm, pmk_ = pmk.next()
                        S.op('dve', lambda e: e.tensor_tensor(out=pm[:, 0:W], in0=px[:, 0:W], in1=mask4[:, 0:W], op=ALU.mult), reads=[pxk, "maskt"], writes=[pmk_])
                        pending.append((u, pm, pmk_))

                    def emit_pv():
                        (u, pm, pmk_) = pending.pop(0)
                        r, nb, p = u
                        b = r * nbd + nb
                        if p % 2 == 0:
                            state["po"] = ps_o.next()
                        po, pok = state["po"]
                        for a in range(2):
                            h = 2 * p + a
                            hs = (h % 4) * 65
                            last = (a == 1)
                            S.op('pe', lambda e: e.matmul(po[:, hs:hs + 65], lhsT=pm[:, a * 128:(a + 1) * 128], rhs=V[:, b, h, 0:65], start=True, stop=(nb == 0)),
                                 reads=[pmk_, (Vk, (b // 8) * 8), (Vk, "ones")], writes=[pok], signal=(nb == 0 and last))
                            if nb > 0:
                                S.op('pe', lambda e: e.matmul(po[:, hs:hs + 65], lhsT=pm[:, 256 + a * 128:256 + (a + 1) * 128], rhs=V[:, b - 1, h, 0:65],
                                                              start=False, stop=True),
                                     reads=[pmk_, (Vk, ((b - 1) // 8) * 8), (Vk, "ones")], writes=[pok], signal=last)
                        if p % 2 == 1:
                            if p == 1:
                                state["os"] = ostg.next()
                            os_, osk = state["os"]
                            if p == 1:
                                S.op('dve', lambda e: e.tensor_copy(out=os_[:, 0:4, :], in_=po[:, 0:260].rearrange("p (h e) -> p h e", h=4)), reads=[pok], writes=[(osk, 0)])
                            else:
                                S.op('act', lambda e: e.copy(out=os_[:, 4:8, :], in_=po[:, 0:260].rearrange("p (h e) -> p h e", h=4)), reads=[pok], writes=[(osk, 1)])
                                S.dma('sp', od_seq[r, nb], os_[:].rearrange("p h e -> p (h e)"), reads=[(osk, 0), (osk, 1)])

                    LAG = 3
                    for ui, u in enumerate(units):
                        emit_qk(u)
                        if len(pending) > LAG:
                            emit_pv()
                        if ui == len(units) // 2 and pi + 1 < len(PATTERNS):
                            nextV = load_V(PATTERNS[pi + 1])
                    while pending:
                        emit_pv()
                S.barrier()

        def phase_E(l, s, xsrc, xdst):
            t0 = s * S_TOK
            with ExitStack() as es:
                Wo = sbt(es, "E_Wo", [128, 12, 1024], BF16)
                S.dma('sp', Wo[:].rearrange("p m n -> p (m n)"), ws_out[l], writes=["Wo"])
                xt = Rot([(sbt(es, "E_xt%d" % i, [128, 1024], F32), "E_xt%d" % i) for i in range(3)])
                hmt = Rot([(sbt(es, "E_hm%d" % i, [128, 1024], BF16), "E_hm%d" % i) for i in range(3)])
                o3 = Rot([(sbt(es, "E_o3%d" % i, [128, 3, 520], F32), "E_o3%d" % i) for i in range(3)])
                azt = Rot([(sbt(es, "E_az%d" % i, [128, 512], BF16), "E_az%d" % i) for i in range(3)])
                osum = Rot([(sbt(es, "E_os%d" % i, [128, 520], F32), "E_os%d" % i) for i in range(2)])
                rden = Rot([(sbt(es, "E_rd%d" % i, [128, 8], F32), "E_rd%d" % i) for i in range(2)])
                ha = Rot([(sbt(es, "E_ha%d" % i, [128, 512], F32), "E_ha%d" % i) for i in range(2)])
                ga = Rot([(sbt(es, "E_ga%d" % i, [128, 512], BF16), "E_ga%d" % i) for i in range(2)])
                gT = Rot([(sbt(es, "E_gT%d" % i, [128, 12, 128], BF16), "E_gT%d" % i) for i in range(2)])
                xo = Rot([(sbt(es, "E_xo%d" % i, [128, 1024], F32), "E_xo%d" % i) for i in range(2)])
                ptr = Rot([(pst(es, "E_ptr%d" % i, [128, 1024], BF16), "E_ptr%d" % i) for i in range(2)])
                ptr2 = Rot([(pst(es, "E_pt2%d" % i, [128, 1024], BF16), "E_pt2%d" % i) for i in range(2)])
                py = Rot([(pst(es, "E_py%d" % i, [128, 512], F32), "E_py%d" % i) for i in range(4)])
                NTL = S_TOK // 128
                jobs = []
                if l + 1 < DEPTH:
                    allp = weight_pieces(l + 1)
                    per = (len(allp) + NSEQ - 1) // NSEQ
                    jobs = allp[s * per:(s + 1) * per]
                    cst = Caster(es, l + 1, "E_pl")

                def load(i):
                    rows = slice(t0 + i * 128, t0 + (i + 1) * 128)
                    a = xt.next(); b = hmt.next(); c = o3.next(); dd = azt.next()
                    S.dma('sp', a[0][:], xsrc[rows, :], writes=[a[1]])
                    S.dma('sp', b[0][:], s_hm[rows, :], writes=[b[1]])
                    S.dma('sp', c[0][:], s_od[:, rows, :].rearrange("g p n -> p g n"), writes=[c[1]])
                    S.dma('sp', dd[0][:], s_az[rows, :], writes=[dd[1]])
                    return a, b, c, dd

                def prep(ld_):
                    (xtt, xtk), (hmtt, hmk), (o3t, o3k), (azz, azk) = ld_
                    os_, osk = osum.next()
                    S.op('dve', lambda e: e.tensor_tensor(out=os_[:], in0=o3t[:, 0, :], in1=o3t[:, 1, :], op=ALU.add), reads=[o3k], writes=[osk])
                    S.op('dve', lambda e: e.tensor_tensor(out=os_[:], in0=os_[:], in1=o3t[:, 2, :], op=ALU.add), reads=[o3k, osk], writes=[osk])
                    osv = os_[:].rearrange("p (h e) -> p h e", h=8)
                    rd, rdk = rden.next()
                    S.op('dve', lambda e: e.reciprocal(out=rd[:].rearrange("p (h o) -> p h o", o=1), in_=osv[:, :, 64:65]), reads=[osk], writes=[rdk])
                    hat, hak = ha.next()
                    S.op('dve', lambda e: e.tensor_tensor(out=hat[:].rearrange("p (h e) -> p h e", h=8), in0=osv[:, :, 0:64],
                                                          in1=rd[:].rearrange("p (h o) -> p h o", o=1).broadcast_to([128, 8, 64]), op=ALU.mult),
                         reads=[osk, rdk], writes=[hak])
                    gat, gak = ga.next()
                    S.op('pool', lambda e: e.tensor_tensor(out=gat[:], in0=hat[:], in1=azz[:], op=ALU.mult), reads=[hak, azk], writes=[gak])
                    gTt, gTk = gT.next()
                    p1, p1k = ptr.next()
                    for mc in range(8):
                        S.op('pe', lambda e: e.transpose(p1[:, mc * 128:(mc + 1) * 128], hmtt[:, mc * 128:(mc + 1) * 128], identb[:]),
                             reads=[hmk, "identb"], writes=[p1k], signal=(mc == 7))
                    S.op('act', lambda e: e.copy(out=gTt[:, 0:8, :], in_=p1[:].rearrange("p (m t) -> p m t", m=8)), reads=[p1k], writes=[(gTk, 0)])
                    p2, p2k = ptr2.next()
                    for mc in range(4):
                        S.op('pe', lambda e: e.transpose(p2[:, mc * 128:(mc + 1) * 128], gat[:, mc * 128:(mc + 1) * 128], identb[:]),
                             reads=[gak, "identb"], writes=[p2k], signal=(mc == 3))
                    S.op('act', lambda e: e.copy(out=gTt[:, 8:12, :], in_=p2[:, 0:512].rearrange("p (m t) -> p m t", m=4)), reads=[p2k], writes=[(gTk, 1)])
                    return (gTt, gTk, xtt, xtk)

                def project(i, pr):
                    (gTt, gTk, xtt, xtk) = pr
                    rows = slice(t0 + i * 128, t0 + (i + 1) * 128)
                    xot, xok = xo.next()
                    for half in range(2):
                        pyt, pyk = py.next()
                        for mc in range(12):
                            S.op('pe', lambda e: e.matmul(pyt[:], lhsT=gTt[:, mc, :], rhs=Wo[:, mc, half * 512:(half + 1) * 512], start=(mc == 0), stop=(mc == 11)),
                                 reads=[(gTk, 0), (gTk, 1), "Wo"], writes=[pyk], signal=(mc == 11))
                        S.op('dve', lambda e: e.tensor_tensor(out=xot[:, half * 512:(half + 1) * 512], in0=xtt[:, half * 512:(half + 1) * 512], in1=pyt[:], op=ALU.add),
                             reads=[xtk, pyk], writes=[(xok, half)])
                    S.dma('sp', xdst[rows, :], xot[:], reads=[(xok, 0), (xok, 1)])

                lds = {0: load(0)}
                if NTL > 1:
                    lds[1] = load(1)
                prs = {0: prep(lds[0])}
                for i in range(NTL):
                    if i + 2 < NTL:
                        lds[i + 2] = load(i + 2)
                    if i + 1 < NTL:
                        prs[i + 1] = prep(lds[i + 1])
                    project(i, prs.pop(i))
                    for _ in range(2):
                        if jobs and (i % 2 == 1 or len(jobs) > NTL - i):
                            cst.run(jobs.pop(0))
                while jobs:
                    cst.run(jobs.pop(0))
                if l + 1 < DEPTH:
                    cst.flush()
                S.barrier()

        prologue()
        for l in range(DEPTH):
            xsrc = x_in if l == 0 else xs[(l - 1) % 2]
            xdst = y_out if l == DEPTH - 1 else xs[l % 2]
            for s in range(NSEQ):
                phase_A(l, s, xsrc)
                phase_B(l, s)
                phase_C(l, s)
                phase_D(l, s)
                phase_E(l, s, xsrc, xdst)
        S.barrier()
        stats = dict(n_ins=S.n_ins, n_wait=S.n_wait, cnt=dict(S.cnt))
    return nc, stats


def host_constants():
    k = np.arange(128)[:, None]
    q = np.arange(128)[None, :]
    mask = np.concatenate([(k <= q), (k >= q), (k <= q), (k <= q), (k >= q), (k >= q)], axis=1).astype(np.float32).astype(NPBF)
    blk = (np.arange(128)[:, None] // 64 == np.arange(128)[None, :] // 64).astype(np.float32).astype(NPBF)
    coef = np.zeros((128, 4), np.float32)
    coef[0::32, 0] = 1.0
    coef[1::32, 1] = 1.0
    coef[0::32, 2] = -0.5 * np.log(128.0)
    coef[:, 3] = EPS
    return {"c_identb": np.eye(128, dtype=np.float32).astype(NPBF), "c_identf": np.eye(128, dtype=np.float32),
            "c_mask": mask, "c_blk": blk, "c_coef": coef}


def layout_params(norm_g, w_in, gate_b, conv_w, conv_b, m_norm_g, q_norm_g, k_norm_g):
    DEPTH = norm_g.shape[0]
    normg = np.ascontiguousarray(norm_g.reshape(DEPTH, 8, 128).transpose(0, 2, 1))
    convw = np.ascontiguousarray(conv_w.reshape(DEPTH, 4, 8, 128).transpose(0, 3, 2, 1))
    convb = np.ascontiguousarray(conv_b.reshape(DEPTH, 8, 128).transpose(0, 2, 1))
    gateb = np.zeros((DEPTH, 128, 2), np.float32)
    w_g = np.zeros((DEPTH, D, 256), np.float32)
    for h in range(4):
        for r in range(3):
            gateb[:, 32 * h + r, 0] = gate_b[:, h]
            gateb[:, 32 * h + r, 1] = gate_b[:, 4 + h]
            w_g[:, :, 32 * h + r] = w_in[:, :, 4096 + h]
            w_g[:, :, 128 + 32 * h + r] = w_in[:, :, 4100 + h]
    qkg = np.stack([np.tile(q_norm_g, (1, 2)), np.tile(k_norm_g, (1, 2))], axis=-1).astype(np.float32)
    mng = np.ascontiguousarray(np.broadcast_to(m_norm_g[:, None, :], (DEPTH, 128, 1024))).astype(np.float32)
    return dict(normg=normg, convw=convw, convb=convb, gateb=gateb, w_g=w_g, qkg=np.ascontiguousarray(qkg), mng=mng)


_CACHE = {}


def run(x, norm_g, w_in, gate_b, conv_w, conv_b, m_norm_g, q_norm_g, k_norm_g, w_out, n_cores, debug=False):
    B, S_TOK, _ = x.shape
    DEPTH = norm_g.shape[0]
    NSEQ = B // n_cores
    key = (S_TOK, NSEQ, DEPTH, debug)
    if key not in _CACHE:
        _CACHE[key] = build_program(S_TOK, NSEQ, DEPTH, debug)
    nc, stats = _CACHE[key]
    shared = dict(host_constants())
    shared.update(layout_params(norm_g, w_in, gate_b, conv_w, conv_b, m_norm_g, q_norm_g, k_norm_g))
    shared["w_in"] = np.ascontiguousarray(w_in, dtype=np.float32)
    shared["w_out"] = np.ascontiguousarray(w_out, dtype=np.float32)
    in_maps = []
    for c in range(n_cores):
        m = dict(shared)
        m["x"] = np.ascontiguousarray(x[c * NSEQ:(c + 1) * NSEQ].reshape(NSEQ * S_TOK, D), dtype=np.float32)
        in_maps.append(m)
    res = run_bass_kernel_spmd(nc, in_maps, core_ids=list(range(n_cores)))
    outs = [np.asarray(r["y"]).reshape(NSEQ, S_TOK, D) for r in res.results]
    return np.concatenate(outs, axis=0).astype(np.float32), res, stats


def kernel(x, norm_g, w_in, gate_b, conv_w, conv_b, m_norm_g, q_norm_g, k_norm_g, w_out):
    args = [np.asarray(a) for a in (x, norm_g, w_in, gate_b, conv_w, conv_b, m_norm_g, q_norm_g, k_norm_g, w_out)]
    out, _, _ = run(*args, n_cores=8)
    return out
```
